# Optimizing a Trainium2 kernel written in Bass

```python
import math
import jax, jax.numpy as jnp
from jax import lax
import numpy as np

D_MODEL = 2048
BATCH = 2
SEQ = 4096
DEPTH = 1

MIX_WIDTH = D_MODEL
POOL_WIDTH = MIX_WIDTH // 2
ATTN_WIDTH = MIX_WIDTH - POOL_WIDTH
POOL_WINDOWS = (2, 4, 8, 16)
N_POOL_GROUPS = len(POOL_WINDOWS)
POOL_GROUP = POOL_WIDTH // N_POOL_GROUPS
DIFF_HEAD_DIM = 64
DIFF_V_DIM = 2 * DIFF_HEAD_DIM
N_DIFF_HEADS = ATTN_WIDTH // DIFF_V_DIM
Q_COLS = N_DIFF_HEADS * 2 * DIFF_HEAD_DIM
K_COLS = N_DIFF_HEADS * 2 * DIFF_HEAD_DIM
V_COLS = N_DIFF_HEADS * DIFF_V_DIM
IN_COLS = POOL_WIDTH + Q_COLS + K_COLS + V_COLS
Q_BLOCK = 128
ROPE_THETA = 10000.0
N_EXPERTS = 16
EC_CAPACITY_FACTOR = 2
D_FF = 128 * ((8 * D_MODEL // 3 + 127) // 128)
LN_EPS = 1e-5
RMS_EPS = 1e-5
DEEPNORM_ALPHA = (2.0 * DEPTH) ** 0.25
DEEPNORM_BETA = (8.0 * DEPTH) ** -0.25

kernel_name = "hybrid_pool_diffattn_ec_moe_encoder"


def lambda_init(layer_idx):
    return 0.8 - 0.6 * math.exp(-0.3 * layer_idx)


def layer_norm(x, g, b):
    xf = x.astype(jnp.float32)
    mu = jnp.mean(xf, axis=-1, keepdims=True)
    var = jnp.mean(jnp.square(xf - mu), axis=-1, keepdims=True)
    y = (xf - mu) * lax.rsqrt(var + LN_EPS) * g.astype(jnp.float32) + b.astype(jnp.float32)
    return y.astype(x.dtype)


def rms_norm(x, g):
    xf = x.astype(jnp.float32)
    y = xf * lax.rsqrt(jnp.mean(jnp.square(xf), axis=-1, keepdims=True) + RMS_EPS)
    return (y * g.astype(jnp.float32)).astype(x.dtype)


def rope_tables(seq, dim, dtype):
    pos = jnp.arange(seq, dtype=jnp.float32)
    inv_freq = ROPE_THETA ** (-jnp.arange(0, dim, 2, dtype=jnp.float32) / dim)
    ang = pos[:, None] * inv_freq[None, :]
    ang = jnp.concatenate([ang, ang], axis=-1)
    return jnp.cos(ang).astype(dtype), jnp.sin(ang).astype(dtype)


def apply_rope(t, cos, sin):
    half = t.shape[-1] // 2
    rot = jnp.concatenate([-t[..., half:], t[..., :half]], axis=-1)
    c = cos[None, :, None, None, :]
    s = sin[None, :, None, None, :]
    return t * c + rot * s


def centred_mean_minus_self(u, window):
    B, S, C = u.shape
    uf = u.astype(jnp.float32)
    cs = jnp.concatenate([jnp.zeros((B, 1, C), jnp.float32), jnp.cumsum(uf, axis=1)], axis=1)
    t = jnp.arange(S)
    lo = jnp.clip(t - window // 2, 0, S)
    hi = jnp.clip(t + window - window // 2, 0, S)
    total = cs[:, hi] - cs[:, lo]
    count = (hi - lo).astype(jnp.float32)[None, :, None]
    return (total / count - uf).astype(u.dtype)


def multiscale_pool_mixer(u, pool_w, pool_scale):
    B, S, _ = u.shape
    ug = u.reshape(B, S, N_POOL_GROUPS, POOL_GROUP)
    pooled = jnp.stack(
        [centred_mean_minus_self(ug[:, :, gi], w) for gi, w in enumerate(POOL_WINDOWS)], axis=2)
    mixed = jnp.einsum('bsgc,gcd->bsgd', pooled, pool_w)
    return mixed.reshape(B, S, POOL_WIDTH) * pool_scale


def diff_attention(q, k, v, cos, sin, lq1, lk1, lq2, lk2, subln_g, lam_init):
    B, S, _ = q.shape
    H, d = N_DIFF_HEADS, DIFF_HEAD_DIM
    q = apply_rope(q.reshape(B, S, H, 2, d), cos, sin) * (d ** -0.5)
    k = apply_rope(k.reshape(B, S, H, 2, d), cos, sin)
    v = v.reshape(B, S, H, DIFF_V_DIM).transpose(0, 2, 1, 3)
    q = q.transpose(0, 2, 3, 1, 4)
    k = k.transpose(0, 2, 3, 1, 4)
    lam = (jnp.exp(jnp.sum(lq1.astype(jnp.float32) * lk1.astype(jnp.float32)))
           - jnp.exp(jnp.sum(lq2.astype(jnp.float32) * lk2.astype(jnp.float32)))
           + lam_init)
    n_blocks = S // Q_BLOCK
    qb = q.reshape(B, H, 2, n_blocks, Q_BLOCK, d).transpose(3, 0, 1, 2, 4, 5)

    def block(q_blk):
        scores = jnp.einsum('bhcqd,bhckd->bhcqk', q_blk, k).astype(jnp.float32)
        probs = jax.nn.softmax(scores, axis=-1)
        diff = (probs[:, :, 0] - lam * probs[:, :, 1]).astype(v.dtype)
        return jnp.einsum('bhqk,bhkv->bhqv', diff, v)

    out = lax.map(block, qb)
    out = out.transpose(1, 0, 3, 2, 4).reshape(B, S, H, DIFF_V_DIM)
    out = rms_norm(out, subln_g) * (1.0 - lam_init)
    return out.reshape(B, S, ATTN_WIDTH)


def expert_choice_ffn(x, w_router, w_gate, w_up, w_down):
    B, S, D = x.shape
    cap = EC_CAPACITY_FACTOR * S // N_EXPERTS
    logits = jnp.einsum('bsd,de->bse', x, w_router).astype(jnp.float32)
    affinity = jax.nn.softmax(logits, axis=-1)
    gate, idx = lax.top_k(affinity.transpose(0, 2, 1), cap)
    xin = jax.vmap(lambda xb, ib: xb[ib])(x, idx)
    h = jax.nn.silu(jnp.einsum('becd,edf->becf', xin, w_gate)) * jnp.einsum('becd,edf->becf', xin, w_up)
    y = jnp.einsum('becf,efd->becd', h, w_down) * gate[..., None].astype(x.dtype)
    return jax.vmap(lambda yb, ib: jnp.zeros((S, D), yb.dtype).at[ib.reshape(-1)].add(yb.reshape(-1, D)))(y, idx)


def setup_inputs(seed: int = 0) -> dict:
    key = jax.random.key(seed)
    ks = jax.random.split(key, 20)
    f32 = jnp.float32
    nrm = lambda k, shape, s: jax.random.normal(k, shape, f32) * s
    x = jax.random.normal(ks[0], (BATCH, SEQ, D_MODEL), f32)
    w_in = nrm(ks[1], (DEPTH, D_MODEL, IN_COLS), D_MODEL ** -0.5)
    v_scale = jnp.concatenate([jnp.ones((IN_COLS - V_COLS,), f32), jnp.full((V_COLS,), DEEPNORM_BETA, f32)])
    w_in = w_in * v_scale
    pool_w = nrm(ks[2], (DEPTH, N_POOL_GROUPS, POOL_GROUP, POOL_GROUP), POOL_GROUP ** -0.5)
    pool_scale = 1.0 + nrm(ks[3], (DEPTH, POOL_WIDTH), 0.1)
    lambda_q1 = nrm(ks[4], (DEPTH, DIFF_HEAD_DIM), 0.1)
    lambda_k1 = nrm(ks[5], (DEPTH, DIFF_HEAD_DIM), 0.1)
    lambda_q2 = nrm(ks[6], (DEPTH, DIFF_HEAD_DIM), 0.1)
    lambda_k2 = nrm(ks[7], (DEPTH, DIFF_HEAD_DIM), 0.1)
    subln_g = 1.0 + nrm(ks[8], (DEPTH, DIFF_V_DIM), 0.02)
    w_out = nrm(ks[9], (DEPTH, MIX_WIDTH, D_MODEL), MIX_WIDTH ** -0.5 * DEEPNORM_BETA)
    ln1_g = 1.0 + nrm(ks[10], (DEPTH, D_MODEL), 0.02)
    ln1_b = nrm(ks[11], (DEPTH, D_MODEL), 0.02)
    w_router = nrm(ks[12], (DEPTH, D_MODEL, N_EXPERTS), D_MODEL ** -0.5)
    w_gate = nrm(ks[13], (DEPTH, N_EXPERTS, D_MODEL, D_FF), D_MODEL ** -0.5)
    w_up = nrm(ks[14], (DEPTH, N_EXPERTS, D_MODEL, D_FF), D_MODEL ** -0.5)
    w_down = nrm(ks[15], (DEPTH, N_EXPERTS, D_FF, D_MODEL), D_FF ** -0.5 * DEEPNORM_BETA)
    ln2_g = 1.0 + nrm(ks[16], (DEPTH, D_MODEL), 0.02)
    ln2_b = nrm(ks[17], (DEPTH, D_MODEL), 0.02)
    return {"x": x, "w_in": w_in, "pool_w": pool_w, "pool_scale": pool_scale,
            "lambda_q1": lambda_q1, "lambda_k1": lambda_k1, "lambda_q2": lambda_q2, "lambda_k2": lambda_k2,
            "subln_g": subln_g, "w_out": w_out, "ln1_g": ln1_g, "ln1_b": ln1_b,
            "w_router": w_router, "w_gate": w_gate, "w_up": w_up, "w_down": w_down,
            "ln2_g": ln2_g, "ln2_b": ln2_b}


def reference(x, w_in, pool_w, pool_scale, lambda_q1, lambda_k1, lambda_q2, lambda_k2,
              subln_g, w_out, ln1_g, ln1_b, w_router, w_gate, w_up, w_down, ln2_g, ln2_b):
    B, S, _ = x.shape
    cos, sin = rope_tables(S, DIFF_HEAD_DIM, x.dtype)
    q0 = POOL_WIDTH
    k0 = q0 + Q_COLS
    v0 = k0 + K_COLS
    for l in range(DEPTH):
        u = jnp.einsum('bsd,dc->bsc', x, w_in[l])
        pool_out = multiscale_pool_mixer(u[..., :q0], pool_w[l], pool_scale[l])
        attn_out = diff_attention(u[..., q0:k0], u[..., k0:v0], u[..., v0:], cos, sin,
                                  lambda_q1[l], lambda_k1[l], lambda_q2[l], lambda_k2[l],
                                  subln_g[l], lambda_init(l))
        mixed = jnp.concatenate([pool_out, attn_out], axis=-1)
        mix = jnp.einsum('bsc,cd->bsd', mixed, w_out[l])
        x = layer_norm(DEEPNORM_ALPHA * x + mix, ln1_g[l], ln1_b[l])
        moe = expert_choice_ffn(x, w_router[l], w_gate[l], w_up[l], w_down[l])
        x = layer_norm(DEEPNORM_ALPHA * x + moe, ln2_g[l], ln2_b[l])
    return x
```

```python
import contextlib
import numpy as np
import ml_dtypes
import concourse.bass as bass
import concourse.mybir as mybir
from concourse.bass_utils import run_bass_kernel_spmd

F32 = mybir.dt.float32
BF16 = mybir.dt.bfloat16
I32 = mybir.dt.int32
AF = mybir.ActivationFunctionType
ALU = mybir.AluOpType
AX = mybir.AxisListType

NCORES = 8
D = 2048
S = 4096
TOK = 1024
NF = 43
DFF = 5504
CAP = 512
ALPHA = 2.0 ** 0.25
LAM_INIT = 0.8 - 0.6 * 1.0
LN_EPS = 1e-5
NBIS = 32


class Buf:
    def __init__(self, name):
        self.name = name
        self.base = {}
        self.part = {}
        self.reads = {}
        self.dsem = None
        self.dval = 0


def _merge(dst, src):
    for k, (s, v) in src.items():
        if k not in dst or dst[k][1] < v:
            dst[k] = (s, v)


class Eng:
    def __init__(self, fw, eng, name, is_pe=False):
        self.fw = fw
        self.e = eng
        self.name = name
        self.sem = fw.newsem("s_" + name)
        self.n = 0
        self.waited = {}
        self.is_pe = is_pe
        self.pend = []

    def wait_for(self, deps):
        for k, (s, v) in deps.items():
            if k is self and self.is_pe:
                continue
            if v > self.waited.get(k, 0):
                self.e.wait_ge(s, v)
                self.waited[k] = v


class FW:
    def __init__(self, nc, stack):
        self.nc = nc
        self.stack = stack
        self.nsem = 0
        self.pe = Eng(self, nc.tensor, "pe", is_pe=True)
        self.act = Eng(self, nc.scalar, "act")
        self.dve = Eng(self, nc.vector, "dve")
        self.pool = Eng(self, nc.gpsimd, "pool")
        self.sp = Eng(self, nc.sync, "sp")
        self.engs = [self.pe, self.act, self.dve, self.pool, self.sp]
        self.dbufs = []

    def newsem(self, name):
        self.nsem += 1
        return self.stack.enter_context(self.nc.semaphore(f"{name}_{self.nsem}"))

    def _collect(self, r, w, pw):
        deps = {}
        for b in r:
            _merge(deps, b.base)
            _merge(deps, b.part)
        for b in w:
            _merge(deps, b.base)
            _merge(deps, b.part)
            _merge(deps, b.reads)
        for b in pw:
            if b.reads:
                nb = {}
                _merge(nb, b.base)
                _merge(nb, b.part)
                _merge(nb, b.reads)
                b.base, b.part, b.reads = nb, {}, {}
            _merge(deps, b.base)
        return deps

    def _record(self, tok, r, w, pw):
        k, s, v = tok
        for b in r:
            _merge(b.reads, {k: (s, v)})
        for b in w:
            b.base, b.part, b.reads = {k: (s, v)}, {}, {}
        for b in pw:
            _merge(b.part, {k: (s, v)})

    def op(self, E, fn, r=(), w=(), pw=(), inc=True):
        deps = self._collect(r, w, pw)
        E.wait_for(deps)
        ins = fn()
        if not inc:
            E.pend.append((tuple(r), tuple(w), tuple(pw)))
            return
        E.n += 1
        ins.then_inc(E.sem, 1)
        tok = (E, E.sem, E.n)
        self._record(tok, r, w, pw)
        for (pr, pw_, ppw) in E.pend:
            self._record(tok, pr, pw_, ppw)
        E.pend = []

    def dma(self, E, out, in_, sbuf, r=(), w=(), pw=(), indirect=None, **kw):
        deps = self._collect(r, w, pw)
        E.wait_for(deps)
        if sbuf.dsem is None:
            sbuf.dsem = self.newsem("d_" + sbuf.name)
            self.dbufs.append(sbuf)
        sbuf.dval += 16
        if indirect is None:
            E.e.dma_start(out=out, in_=in_, **kw).then_inc(sbuf.dsem, 16)
        else:
            E.e.indirect_dma_start(out=out, out_offset=None, in_=in_,
                                   in_offset=bass.IndirectOffsetOnAxis(ap=indirect, axis=0)).then_inc(sbuf.dsem, 16)
        tok = (("d", id(sbuf)), sbuf.dsem, sbuf.dval)
        self._record(tok, r, w, pw)

    def coll(self, in_ap, out_ap, r, w):
        E = self.pool
        deps = self._collect(r, w, ())
        E.wait_for(deps)
        sem = self.newsem("cc")
        E.e.collective_compute("AllGather", ALU.bypass, replica_groups=[list(range(NCORES))],
                               ins=[in_ap], outs=[out_ap]).then_inc(sem)
        tok = (("c", id(sem)), sem, 1)
        self._record(tok, r, w, ())

    def barrier(self):
        alld = {}
        for E in self.engs:
            if E.pend:
                raise RuntimeError("pending unsignalled ops on " + E.name)
            if E.n > 0:
                alld[E] = (E.sem, E.n)
        for b in self.dbufs:
            alld[("d", id(b))] = (b.dsem, b.dval)
        for E in self.engs:
            d = {k: v for k, v in alld.items() if k is not E}
            if E.n > 0 and not E.is_pe:
                d[E] = (E.sem, E.n)
            E.wait_for(d)


def build_program(stage=99, dbg=False):
    nc = bass.Bass("TRN2", target_bir_lowering=False)
    top = contextlib.ExitStack()
    fw = FW(nc, top)
    pe, act, dve, pool, sp = fw.pe, fw.act, fw.dve, fw.pool, fw.sp
    V, A, P, T = nc.vector, nc.scalar, nc.gpsimd, nc.tensor

    def din(name, shape, dt=F32):
        return nc.dram_tensor(name, shape, dt, kind="ExternalInput")

    xT = din("xT", [16, 128, 1040])
    xtok = din("xtok", [TOK, D])
    w_in_r = din("w_in_r", [32, 128, 2048])
    poolw_r = din("poolw_r", [128, 8 * 256])
    pscale = din("pscale", [128, 8])
    lam_in = din("lam_in", [128, 256])
    subg = din("subg", [128, 128])
    subgc_d = din("subgc", [128, 1])
    w_out_r = din("w_out_r", [128, 16 * 2048])
    ln1g = din("ln1g", [128, D]); ln1b = din("ln1b", [128, D])
    ln2g = din("ln2g", [128, D]); ln2b = din("ln2b", [128, D])
    wr_r = din("wr_r", [128, 256])
    cosT_d = din("cosT", [128, TOK]); sinT_d = din("sinT", [128, TOK])
    invcnt_d = din("invcnt", [128, 4 * TOK])
    Rm_d = din("Rm", [128, 128]); ident_d = din("ident", [128, 128])
    ltri_d = din("ltri", [128, 128]); ones_d = din("ones", [128, 128])
    iota_d = din("iota512", [128, 512])
    tdec_d = din("tdec", [128, 2 * 32 * 2])
    idxKV_d = din("idxKV", [64, 128, 1], I32)
    sel_d = din("sel", [128, 32])
    coreoff_d = din("coreoff", [128, 1])
    idxPM_d = din("idxPM", [64, 128, 1], I32)
    if stage >= 2:
        wgu_r = din("wgu_r", [2, NF, 128, 4096])
        wd_r = din("wd_r", [2, 4, NF, 128, 512])
    out_d = nc.dram_tensor("out", [TOK, D], F32, kind="ExternalOutput")
    dbg_d = nc.dram_tensor("dbg", [TOK, D], F32, kind="ExternalOutput") if dbg else None

    kvloc = nc.dram_tensor("kvloc", [2048, 1024], BF16)
    kvall = nc.dram_tensor("kvall", [NCORES * 2048, 1024], BF16)
    x1bf = nc.dram_tensor("x1bf", [TOK, D], BF16)
    x1all = nc.dram_tensor("x1all", [NCORES * TOK, D], BF16)
    x1f = nc.dram_tensor("x1f", [TOK, D], F32)
    affloc = nc.dram_tensor("affloc", [TOK, 16], F32)
    affall = nc.dram_tensor("affall", [NCORES * TOK, 16], F32)
    yloc = nc.dram_tensor("yloc", [2048, D], F32)
    yall = nc.dram_tensor("yall", [NCORES * 2048, D], F32)
    pmloc = nc.dram_tensor("pmloc", [2 * S, 4], F32)
    pmall = nc.dram_tensor("pmall", [NCORES * 2 * S, 4], F32)
    B_kvloc, B_kvall = Buf("kvloc"), Buf("kvall")
    B_x1bf, B_x1all, B_x1f = Buf("x1bf"), Buf("x1all"), Buf("x1f")
    B_affloc, B_affall = Buf("affloc"), Buf("affall")
    B_yloc, B_yall, B_pmloc, B_pmall = Buf("yloc"), Buf("yall"), Buf("pmloc"), Buf("pmall")

    def sbt(stack, name, shape, dt):
        return stack.enter_context(nc.sbuf_tensor(name, shape, dt))

    def early_exit(stacks):
        fw.barrier()
        sd = contextlib.ExitStack()
        t_ = sbt(sd, "dbgt", [128, D], F32); Bt = Buf("dbgt")
        for tt in range(8):
            fw.dma(sp, t_[:], xtok[tt * 128:(tt + 1) * 128, :], Bt, w=[Bt])
            fw.dma(sp, out_d[tt * 128:(tt + 1) * 128, :], t_[:], Bt, r=[Bt])
        fw.barrier()
        sd.close()
        for st_ in stacks:
            st_.close()
        top.close()
        return nc

    PS = [top.enter_context(nc.psum_tensor(f"ps{i}", [128, 512], F32)) for i in range(8)]
    BPS = [Buf(f"ps{i}") for i in range(8)]

    if stage == 0:
        sd = contextlib.ExitStack()
        t_ = sbt(sd, "dbgt", [128, D], F32); Bt = Buf("dbgt")
        for tt in range(8):
            fw.dma(sp, t_[:], xtok[tt * 128:(tt + 1) * 128, :], Bt, w=[Bt])
            fw.dma(sp, out_d[tt * 128:(tt + 1) * 128, :], t_[:], Bt, r=[Bt])
        fw.barrier()
        sd.close()
        top.close()
        return nc
    ident_f = sbt(top, "ident_f", [128, 128], F32); B_identf = Buf("identf")
    ident_b = sbt(top, "ident_b", [128, 128], BF16); B_identb = Buf("identb")
    dmy = sbt(top, "dmy", [128, 1], I32)

    def harden(tile, B):
        fw.op(dve, lambda: V.tensor_copy(out=dmy[:], in_=tile[:]), w=[B])
    fw.dma(sp, ident_f[:], ident_d[:, :], B_identf, w=[B_identf])
    fw.dma(pool, ident_b[:], ident_d[:, :], B_identb, w=[B_identb])

    sA = contextlib.ExitStack()
    mixedT = sbt(sA, "mixedT", [128, 16, TOK], BF16)
    B_mixed = [Buf(f"mixed{i}") for i in range(16)]
    qT = sbt(sA, "qT", [128, 8, TOK], BF16)
    B_q = [Buf(f"q{i}") for i in range(8)]

    s1 = contextlib.ExitStack()
    xT_s = sbt(s1, "xT_s", [128, 16, 1040], BF16); B_xT = Buf("xT")
    wring = [sbt(s1, f"wring{i}", [128, 16, 128], BF16) for i in range(4)]
    B_wr = [Buf(f"wring{i}") for i in range(4)]
    cosT = sbt(s1, "cosT_s", [128, TOK], F32); sinT = sbt(s1, "sinT_s", [128, TOK], F32)
    B_cs = Buf("cossin")
    Rm_b = sbt(s1, "Rm_b", [128, 128], BF16); B_Rm = Buf("Rm")
    invcnt = sbt(s1, "invcnt_s", [128, 4, TOK], F32); B_inv = Buf("invcnt")
    poolw = sbt(s1, "poolw_s", [128, 8, 256], BF16); B_poolw = Buf("poolw")
    pscale_s = sbt(s1, "pscale_s", [128, 8], F32); B_pscale = Buf("pscale")
    pooledT = sbt(s1, "pooledT", [128, 8, TOK], BF16); B_pooled = [Buf(f"pooled{i}") for i in range(8)]
    upool = sbt(s1, "upool", [128, 1040], F32); B_upool = Buf("upool")
    wsa = sbt(s1, "wsa", [128, 1040], F32); wsb = sbt(s1, "wsb", [128, 1040], F32)
    B_wsa, B_wsb = Buf("wsa"), Buf("wsb")
    ropea = [sbt(s1, f"ropea{i}", [128, 512], BF16) for i in range(2)]
    ropeb = [sbt(s1, f"ropeb{i}", [128, 512], BF16) for i in range(2)]
    B_ropea = [Buf("ropea0"), Buf("ropea1")]; B_ropeb = [Buf("ropeb0"), Buf("ropeb1")]
    kst = [sbt(s1, f"kst{i}", [128, TOK], BF16) for i in range(2)]; B_kst = [Buf("kst0"), Buf("kst1")]
    vst = [sbt(s1, f"vst{i}", [128, 8, 128], BF16) for i in range(2)]; B_vst = [Buf("vst0"), Buf("vst1")]

    for k in range(16):
        fw.dma(pool, xT_s[:, k, :], xT[k], B_xT, pw=[B_xT])
    fw.dma(sp, cosT[:], cosT_d[:, :], B_cs, pw=[B_cs])
    fw.dma(sp, sinT[:], sinT_d[:, :], B_cs, pw=[B_cs])
    fw.dma(pool, Rm_b[:], Rm_d[:, :], B_Rm, w=[B_Rm])
    fw.dma(sp, invcnt[:].rearrange("p g t -> p (g t)"), invcnt_d[:, :], B_inv, w=[B_inv])
    fw.dma(pool, poolw[:].rearrange("p a b -> p (a b)"), poolw_r[:, :], B_poolw, w=[B_poolw])
    fw.dma(sp, pscale_s[:], pscale[:, :], B_pscale, w=[B_pscale])

    ORDER = list(range(16, 32)) + list(range(0, 16))
    slot_of = {cc: pos % 4 for pos, cc in enumerate(ORDER)}

    def load_w(pos):
        cc = ORDER[pos]
        s = slot_of[cc]
        fw.dma(pool, wring[s][:].rearrange("p k j -> p (k j)"), w_in_r[cc], B_wr[s], w=[B_wr[s]])

    bank_rr = [0]

    def nextbank(lo=0, hi=4):
        b = lo + bank_rr[0] % (hi - lo)
        bank_rr[0] += 1
        return b

    def proj_T(cc, c0, n, bank):
        s = slot_of[cc]
        for k in range(16):
            fw.op(pe, lambda k=k: T.matmul(PS[bank][:, 0:n], wring[s][:, k, :], xT_s[:, k, c0:c0 + n],
                                            start=(k == 0), stop=(k == 15)),
                  r=[B_wr[s], B_xT], w=[BPS[bank]] if k == 0 else (), pw=[BPS[bank]] if k else (),
                  inc=(k == 15))

    load_w(0); load_w(1); load_w(2)
    WIN = (2, 4, 8, 16)
    for pos, cc in enumerate(ORDER):
        if pos + 3 < 32:
            load_w(pos + 3)
        if cc < 8:
            g = cc // 2
            wdw = WIN[g]
            for (c0, n) in ((0, 512), (512, 512), (1024, 16)):
                bk = nextbank()
                proj_T(cc, c0, n, bk)
                fw.op(act, lambda bk=bk, c0=c0, n=n: A.copy(out=upool[:, c0:c0 + n], in_=PS[bk][:, 0:n]),
                      r=[BPS[bk]], pw=[B_upool])
            cur, Bcur = upool, B_upool
            step = 1
            L = 1040
            pp = [(wsa, B_wsa), (wsb, B_wsb)]
            i = 0
            while step < wdw:
                nxt, Bn = pp[i % 2]
                L2 = L - step
                fw.op(dve, lambda cur=cur, nxt=nxt, L2=L2, step=step: V.tensor_tensor(
                    out=nxt[:, 0:L2], in0=cur[:, 0:L2], in1=cur[:, step:step + L2], op=ALU.add),
                    r=[Bcur], w=[Bn])
                cur, Bcur = nxt, Bn
                L = L2
                step *= 2
                i += 1
            o = 8 - wdw // 2
            nxt, Bn = pp[i % 2]
            fw.op(dve, lambda cur=cur, nxt=nxt, o=o, g=g: V.tensor_tensor(
                out=nxt[:, 0:TOK], in0=cur[:, o:o + TOK], in1=invcnt[:, g, :], op=ALU.mult),
                r=[Bcur, B_inv], w=[Bn])
            fw.op(dve, lambda nxt=nxt, cc=cc: V.tensor_tensor(
                out=pooledT[:, cc, :], in0=nxt[:, 0:TOK], in1=upool[:, 8:8 + TOK], op=ALU.subtract),
                r=[Bn, B_upool], w=[B_pooled[cc]])
            if cc == 7:
                for gg in range(4):
                    for dc in range(2):
                        for half in range(2):
                            bk = nextbank()
                            for kc in range(2):
                                fw.op(pe, lambda gg=gg, dc=dc, half=half, kc=kc, bk=bk: T.matmul(
                                    PS[bk][:, :], poolw[:, gg * 2 + kc, dc * 128:(dc + 1) * 128],
                                    pooledT[:, gg * 2 + kc, half * 512:(half + 1) * 512],
                                    start=(kc == 0), stop=(kc == 1)),
                                    r=[B_poolw, B_pooled[gg * 2 + kc]],
                                    w=[BPS[bk]] if kc == 0 else (), pw=[BPS[bk]] if kc else (), inc=(kc == 1))
                            ch = gg * 2 + dc
                            fw.op(dve, lambda ch=ch, half=half, bk=bk: V.tensor_scalar(
                                out=mixedT[:, ch, half * 512:(half + 1) * 512], in0=PS[bk][:, :],
                                scalar1=pscale_s[:, ch:ch + 1], scalar2=None, op0=ALU.mult),
                                r=[BPS[bk], B_pscale], pw=[B_mixed[ch]])
        elif cc < 24:
            isq = cc < 16
            h = cc - 8 if isq else cc - 16
            for half in range(2):
                bk = nextbank()
                proj_T(cc, 8 + half * 512, 512, bk)
                ra, rb = ropea[half], ropeb[half]
                fw.op(dve, lambda bk=bk, ra=ra, half=half: V.tensor_tensor(
                    out=ra[:], in0=PS[bk][:, :], in1=cosT[:, half * 512:(half + 1) * 512], op=ALU.mult),
                    r=[BPS[bk], B_cs], w=[B_ropea[half]])
                fw.op(dve, lambda bk=bk, rb=rb, half=half: V.tensor_tensor(
                    out=rb[:], in0=PS[bk][:, :], in1=sinT[:, half * 512:(half + 1) * 512], op=ALU.mult),
                    r=[BPS[bk], B_cs], w=[B_ropeb[half]])
                b2 = nextbank(4, 8)
                fw.op(pe, lambda b2=b2, ra=ra: T.matmul(PS[b2][:, :], ident_b[:], ra[:], start=True, stop=False),
                      r=[B_identb, B_ropea[half]], w=[BPS[b2]], inc=False)
                fw.op(pe, lambda b2=b2, rb=rb: T.matmul(PS[b2][:, :], Rm_b[:], rb[:], start=False, stop=True),
                      r=[B_Rm, B_ropeb[half]], pw=[BPS[b2]])
                if isq:
                    fw.op(act, lambda b2=b2, h=h, half=half: A.mul(
                        out=qT[:, h, half * 512:(half + 1) * 512], in_=PS[b2][:, :], mul=0.125),
                        r=[BPS[b2]], pw=[B_q[h]])
                else:
                    ks, Bk = kst[h % 2], B_kst[h % 2]
                    fw.op(act, lambda b2=b2, ks=ks, half=half: A.copy(
                        out=ks[:, half * 512:(half + 1) * 512], in_=PS[b2][:, :]),
                        r=[BPS[b2]], pw=[Bk])
            if not isq:
                fw.dma(sp, kvloc[h * 128:(h + 1) * 128, :], kst[h % 2][:], B_kst[h % 2],
                       r=[B_kst[h % 2]], pw=[B_kvloc])
        else:
            h = cc - 24
            s = slot_of[cc]
            vs, Bv = vst[h % 2], B_vst[h % 2]
            for tg in range(2):
                bk = nextbank()
                for t4 in range(4):
                    tt = tg * 4 + t4
                    for k in range(16):
                        fw.op(pe, lambda k=k, tt=tt, t4=t4, bk=bk, s=s: T.matmul(
                            PS[bk][:, t4 * 128:(t4 + 1) * 128], xT_s[:, k, 8 + tt * 128:8 + (tt + 1) * 128],
                            wring[s][:, k, :], start=(k == 0), stop=(k == 15)),
                            r=[B_wr[s], B_xT],
                            w=[BPS[bk]] if (k == 0 and t4 == 0) else (),
                            pw=[BPS[bk]] if not (k == 0 and t4 == 0) else (),
                            inc=(k == 15 and t4 == 3))
                fw.op(act, lambda bk=bk, vs=vs, tg=tg: A.copy(
                    out=vs[:, tg * 4:(tg + 1) * 4, :].rearrange("p a b -> p (a b)"), in_=PS[bk][:, :]),
                    r=[BPS[bk]], pw=[Bv])
            fw.dma(sp, kvloc[1024 + h * 128:1024 + (h + 1) * 128, :], vs[:].rearrange("p a b -> p (a b)"),
                   Bv, r=[Bv], pw=[B_kvloc])
            if cc == 31 and stage != 0.05:
                fw.coll(kvloc[:, :], kvall[:, :], r=[B_kvloc], w=[B_kvall])

    if stage == 0.05:
        return early_exit([s1, sA])
    if stage == 0.1:
        return early_exit([s1, sA])
    fw.barrier()
    s1.close()

    s2 = contextlib.ExitStack()
    kTh = [sbt(s2, f"kTh{i}", [128, S], BF16) for i in range(2)]; B_kTh = [Buf("kTh0"), Buf("kTh1")]
    vh = [sbt(s2, f"vh{i}", [128, 32, 128], BF16) for i in range(2)]; B_vh = [Buf("vh0"), Buf("vh1")]
    pT = [sbt(s2, f"pT{i}", [128, 512], BF16) for i in range(3)]; B_pT = [Buf(f"pT{i}") for i in range(3)]
    ikv = [sbt(s2, f"ikv{c}", [128, 1], I32) for c in range(64)]; B_idxKV = Buf("idxKV")
    lam_s = sbt(s2, "lam_s", [128, 256], F32); B_lam = Buf("lam")
    lamv = sbt(s2, "lamv", [128, 8], F32); B_lamv = Buf("lamv")
    subg_s = sbt(s2, "subg_s", [128, 128], F32); B_subg = Buf("subg")
    ot = [sbt(s2, f"ot{i}", [128, 128], F32) for i in range(4)]; B_ot = [Buf(f"ot{i}") for i in range(4)]
    osm = sbt(s2, "osm", [128, 16], F32); B_osm = Buf("osm")
    ao = sbt(s2, "ao", [128, 128], BF16); B_ao = Buf("ao")
    junk = sbt(s2, "junk", [128, 128], F32); B_junk = Buf("junk")

    for c in range(64):
        fw.dma(sp, ikv[c][:], idxKV_d[c], B_idxKV, pw=[B_idxKV])
    fw.dma(sp, lam_s[:], lam_in[:, :], B_lam, w=[B_lam])
    fw.dma(sp, subg_s[:], subg[:, :], B_subg, w=[B_subg])
    for i in range(2):
        fw.op(dve, lambda i=i: V.tensor_tensor(out=junk[:, 0:64], in0=lam_s[:, i * 128:i * 128 + 64],
                                               in1=lam_s[:, i * 128 + 64:i * 128 + 128], op=ALU.mult),
              r=[B_lam], w=[B_junk])
        fw.op(dve, lambda i=i: V.tensor_reduce(out=lamv[:, i:i + 1], in_=junk[:, 0:64], axis=AX.X, op=ALU.add),
              r=[B_junk], pw=[B_lamv])
    fw.op(act, lambda: A.activation(out=lamv[:, 2:4], in_=lamv[:, 0:2], func=AF.Exp), r=[B_lamv], pw=[B_lamv])
    fw.op(dve, lambda: V.tensor_tensor(out=lamv[:, 4:5], in0=lamv[:, 3:4], in1=lamv[:, 2:3], op=ALU.subtract),
          r=[B_lamv], pw=[B_lamv])
    fw.op(dve, lambda: V.tensor_scalar(out=lamv[:, 5:6], in0=lamv[:, 4:5], scalar1=-LAM_INIT, scalar2=None,
                                       op0=ALU.add), r=[B_lamv], pw=[B_lamv])

    icol = [sbt(s2, f"icol{i}", [128, 1], I32) for i in range(4)]; B_icol = [Buf(f"icol{i}") for i in range(4)]
    vtmp = [sbt(s2, f"vtmp{i}", [128, 1024], BF16) for i in range(2)]; B_vtmp = [Buf("vtmp0"), Buf("vtmp1")]
    ic_i = [0]

    def load_head(h):
        s = h % 2
        for r4 in range(4):
            c = h * 4 + r4
            fw.dma(pool, kTh[s][:, r4 * 1024:(r4 + 1) * 1024], kvall[:, :], B_kTh[s],
                   r=[B_kvall, B_idxKV], pw=[B_kTh[s]], indirect=ikv[c][:, :])
            fw.dma(pool, vh[s][:, r4 * 8:(r4 + 1) * 8, :].rearrange("p a b -> p (a b)"), kvall[:, :], B_vh[s],
                   r=[B_kvall, B_idxKV], pw=[B_vh[s]], indirect=ikv[32 + c][:, :])

    load_head(0)
    if stage == 0.15:
        return early_exit([s2, sA])
    SB = (0, 1, 2)
    ones_b = sbt(s2, "ones_b", [128, 128], BF16); B_onesb = Buf("onesb")
    ones_f = sbt(s2, "ones_f", [128, 128], F32); B_onesf = Buf("onesf")
    fw.op(dve, lambda: V.memset(ones_b[:], 1.0), w=[B_onesb])
    fw.op(dve, lambda: V.memset(ones_f[:], 1.0), w=[B_onesf])
    subgc = sbt(s2, "subgc_s", [128, 1], F32); B_subgc = Buf("subgc")
    fw.dma(sp, subgc[:], subgc_d[:, :], B_subgc, w=[B_subgc])
    fin = [sbt(s2, f"fin{i}", [128, 512], F32) for i in range(6)]; B_fin = [Buf(f"fin{i}") for i in range(6)]
    sqh = sbt(s2, "sqh", [128, 512], BF16); B_sqh = Buf("sqh")
    sql = sbt(s2, "sql", [128, 512], BF16); B_sql = Buf("sql")
    sc_i = [0]
    for h in range(8):
        if h + 1 < 8:
            load_head(h + 1)
        s = h % 2
        for qh in range(2):
            for c in range(2):
                for kt in range(32):
                    i = sc_i[0]; sc_i[0] += 1
                    sb_ = SB[i % 3]
                    pt, Bp = pT[i % 3], B_pT[i % 3]
                    fw.op(pe, lambda sb_=sb_, c=c, kt=kt, s=s, h=h, qh=qh: T.matmul(
                        PS[sb_][:, :], kTh[s][c * 64:(c + 1) * 64, kt * 128:(kt + 1) * 128],
                        qT[c * 64:(c + 1) * 64, h, qh * 512:(qh + 1) * 512], start=True, stop=True),
                        r=[B_kTh[s], B_q[h]], w=[BPS[sb_]])
                    fw.op(act, lambda sb_=sb_, pt=pt: A.activation(out=pt[:], in_=PS[sb_][:, :], func=AF.Exp),
                          r=[BPS[sb_]], w=[Bp])
                    fw.op(pe, lambda c=c, pt=pt, kt=kt, s=s: T.matmul(
                        PS[3 + c][:, :], vh[s][:, kt, :], pt[:], start=(kt == 0), stop=(kt == 31)),
                        r=[Bp, B_vh[s]], w=[BPS[3 + c]] if kt == 0 else (), pw=[BPS[3 + c]] if kt else (),
                        inc=False)
                    fw.op(pe, lambda c=c, pt=pt, kt=kt: T.matmul(
                        PS[5 + c][:, :], ones_b[:], pt[:], start=(kt == 0), stop=(kt == 31)),
                        r=[Bp, B_onesb], w=[BPS[5 + c]] if kt == 0 else (), pw=[BPS[5 + c]] if kt else ())
            rc0, rc1, o0, t_, o_, sq = fin
            Brc0, Brc1, Bo0, Bt_, Bo_, Bsq = B_fin
            fw.op(dve, lambda: V.reciprocal(out=rc0[:], in_=PS[5][:, :]), r=[BPS[5]], w=[Brc0])
            fw.op(dve, lambda: V.reciprocal(out=rc1[:], in_=PS[6][:, :]), r=[BPS[6]], w=[Brc1])
            fw.op(dve, lambda: V.tensor_tensor(out=o0[:], in0=PS[3][:, :], in1=rc0[:], op=ALU.mult),
                  r=[BPS[3], Brc0], w=[Bo0])
            fw.op(dve, lambda: V.tensor_tensor(out=t_[:], in0=PS[4][:, :], in1=rc1[:], op=ALU.mult),
                  r=[BPS[4], Brc1], w=[Bt_])
            fw.op(dve, lambda: V.scalar_tensor_tensor(out=o_[:], in0=t_[:], scalar=lamv[:, 5:6], in1=o0[:],
                                                      op0=ALU.mult, op1=ALU.add),
                  r=[Bt_, Bo0, B_lamv], w=[Bo_])
            fw.op(dve, lambda: V.tensor_tensor(out=sq[:], in0=o_[:], in1=o_[:], op=ALU.mult), r=[Bo_], w=[Bsq])
            fw.op(dve, lambda: V.tensor_copy(out=sqh[:], in_=sq[:]), r=[Bsq], w=[B_sqh])
            fw.op(dve, lambda: V.tensor_tensor(out=sql[:], in0=sq[:], in1=sqh[:], op=ALU.subtract),
                  r=[Bsq, B_sqh], w=[B_sql])
            fw.op(pe, lambda: T.matmul(PS[7][:, :], ones_b[:], sqh[:], start=True, stop=False),
                  r=[B_onesb, B_sqh], w=[BPS[7]], inc=False)
            fw.op(pe, lambda: T.matmul(PS[7][:, :], ones_b[:], sql[:], start=False, stop=True),
                  r=[B_onesb, B_sql], pw=[BPS[7]])
            fw.op(dve, lambda: V.tensor_scalar(out=rc0[:], in0=PS[7][:, :], scalar1=1.0 / 128.0, scalar2=1e-5,
                                               op0=ALU.mult, op1=ALU.add), r=[BPS[7]], w=[Brc0])
            fw.op(act, lambda: A.sqrt(out=rc1[:], in_=rc0[:]), r=[Brc0], w=[Brc1])
            fw.op(dve, lambda: V.reciprocal(out=rc0[:], in_=rc1[:]), r=[Brc1], w=[Brc0])
            fw.op(dve, lambda: V.tensor_tensor(out=t_[:], in0=o_[:], in1=rc0[:], op=ALU.mult),
                  r=[Bo_, Brc0], w=[Bt_])
            fw.op(dve, lambda h=h, qh=qh: V.tensor_scalar(
                out=mixedT[:, 8 + h, qh * 512:(qh + 1) * 512], in0=t_[:], scalar1=subgc[:, 0:1],
                scalar2=(1.0 - LAM_INIT), op0=ALU.mult, op1=ALU.mult),
                r=[Bt_, B_subgc], pw=[B_mixed[8 + h]])

    if stage == 0.2:
        return early_exit([s2, sA])
    fw.barrier()
    s2.close()

    s3 = contextlib.ExitStack()
    wout = sbt(s3, "wout_s", [128, 16, D], BF16); B_wout = Buf("wout")
    g1 = sbt(s3, "g1", [128, D], F32); b1 = sbt(s3, "b1", [128, D], F32); B_ln1 = Buf("ln1")
    wr_s = sbt(s3, "wr_s", [128, 16, 16], F32); B_wrs = Buf("wr")
    xt_ = [sbt(s3, f"xt{i}", [128, D], F32) for i in range(2)]; B_xt = [Buf("xt0"), Buf("xt1")]
    rt = [sbt(s3, f"rt{i}", [128, D], F32) for i in range(2)]; B_rt = [Buf("rt0"), Buf("rt1")]
    x1b = [sbt(s3, f"x1b{i}", [128, D], BF16) for i in range(2)]; B_x1b = [Buf("x1b0"), Buf("x1b1")]
    x1T = sbt(s3, "x1T", [128, 16, 128], F32); B_x1T = Buf("x1T")
    stats = sbt(s3, "stats", [128, 4, 6], F32); B_stats = Buf("stats")
    mv = sbt(s3, "mv", [128, 8], F32); B_mv = Buf("mv")
    aff_s = sbt(s3, "aff_s", [128, 8, 16], F32); B_affs = Buf("affs")
    lsm = sbt(s3, "lsm", [128, 4], F32); B_lsm = Buf("lsm")

    for k in range(16):
        fw.dma(pool, wout[:, k, :], w_out_r[:, k * D:(k + 1) * D], B_wout, pw=[B_wout])
    fw.dma(sp, g1[:], ln1g[:, :], B_ln1, pw=[B_ln1])
    fw.dma(sp, b1[:], ln1b[:, :], B_ln1, pw=[B_ln1])
    fw.dma(sp, wr_s[:].rearrange("p a b -> p (a b)"), wr_r[:, :], B_wrs, w=[B_wrs])

    def layer_norm(r_t, B_r, gam, bet, B_gb, out_t, B_out):
        for c4 in range(4):
            fw.op(dve, lambda c4=c4: V.bn_stats(out=stats[:, c4, :], in_=r_t[:, c4 * 512:(c4 + 1) * 512]),
                  r=[B_r], pw=[B_stats])
        fw.op(dve, lambda: V.bn_aggr(out=mv[:, 0:2], in_=stats[:].rearrange("p a b -> p (a b)")),
              r=[B_stats], pw=[B_mv])
        fw.op(dve, lambda: V.tensor_scalar(out=mv[:, 2:3], in0=mv[:, 1:2], scalar1=LN_EPS, scalar2=None,
                                           op0=ALU.add), r=[B_mv], pw=[B_mv])
        fw.op(act, lambda: A.sqrt(out=mv[:, 3:4], in_=mv[:, 2:3]), r=[B_mv], pw=[B_mv])
        fw.op(dve, lambda: V.reciprocal(out=mv[:, 4:5], in_=mv[:, 3:4]), r=[B_mv], pw=[B_mv])
        fw.op(dve, lambda: V.tensor_scalar(out=r_t[:], in0=r_t[:], scalar1=mv[:, 0:1], scalar2=mv[:, 4:5],
                                           op0=ALU.subtract, op1=ALU.mult), r=[B_mv], w=[B_r])
        fw.op(dve, lambda: V.tensor_tensor(out=r_t[:], in0=r_t[:], in1=gam[:], op=ALU.mult),
              r=[B_gb], w=[B_r])
        fw.op(dve, lambda: V.tensor_tensor(out=out_t[:], in0=r_t[:], in1=bet[:], op=ALU.add),
              r=[B_gb, B_r], w=[B_out])

    for tt in range(8):
        xs, Bx = xt_[tt % 2], B_xt[tt % 2]
        rr, Br = rt[tt % 2], B_rt[tt % 2]
        fw.dma(sp, xs[:], xtok[tt * 128:(tt + 1) * 128, :], Bx, w=[Bx])
        for dc in range(4):
            for k in range(16):
                fw.op(pe, lambda k=k, dc=dc, tt=tt: T.matmul(
                    PS[dc][:, :], mixedT[:, k, tt * 128:(tt + 1) * 128], wout[:, k, dc * 512:(dc + 1) * 512],
                    start=(k == 0), stop=(k == 15)),
                    r=[B_mixed[k], B_wout], w=[BPS[dc]] if k == 0 else (), pw=[BPS[dc]] if k else (),
                    inc=(k == 15))
            fw.op(dve, lambda dc=dc, xs=xs, rr=rr: V.scalar_tensor_tensor(
                out=rr[:, dc * 512:(dc + 1) * 512], in0=xs[:, dc * 512:(dc + 1) * 512], scalar=ALPHA,
                in1=PS[dc][:, :], op0=ALU.mult, op1=ALU.add),
                r=[Bx, BPS[dc]], pw=[Br])
        layer_norm(rr, Br, g1, b1, B_ln1, xs, Bx)
        fw.dma(sp, x1f[tt * 128:(tt + 1) * 128, :], xs[:], Bx, r=[Bx], pw=[B_x1f])
        xb, Bxb = x1b[tt % 2], B_x1b[tt % 2]
        fw.op(act, lambda xb=xb, xs=xs: A.copy(out=xb[:], in_=xs[:]), r=[Bx], w=[Bxb])
        fw.dma(sp, x1bf[tt * 128:(tt + 1) * 128, :], xb[:], Bxb, r=[Bxb], pw=[B_x1bf])
        for k4 in range(4):
            tb = 4 + (k4 % 2)
            for j in range(4):
                k = k4 * 4 + j
                fw.op(pe, lambda tb=tb, j=j, k=k, xs=xs: T.transpose(
                    PS[tb][:, j * 128:(j + 1) * 128], xs[:, k * 128:(k + 1) * 128], ident_f[:]),
                    r=[Bx, B_identf], w=[BPS[tb]] if j == 0 else (), pw=[BPS[tb]] if j else (), inc=(j == 3))
            fw.op(act, lambda tb=tb, k4=k4: A.copy(
                out=x1T[:, k4 * 4:(k4 + 1) * 4, :].rearrange("p a b -> p (a b)"), in_=PS[tb][:, :]),
                r=[BPS[tb]], pw=[B_x1T])
        for k in range(16):
            fw.op(pe, lambda k=k: T.matmul(PS[6][:, 0:16], x1T[:, k, :], wr_s[:, k, :],
                                           start=(k == 0), stop=(k == 15)),
                  r=[B_x1T, B_wrs], w=[BPS[6]] if k == 0 else (), pw=[BPS[6]] if k else (), inc=(k == 15))
        fw.op(dve, lambda: V.tensor_reduce(out=lsm[:, 0:1], in_=PS[6][:, 0:16], axis=AX.X, op=ALU.max),
              r=[BPS[6]], pw=[B_lsm])
        fw.op(dve, lambda: V.tensor_scalar(out=lsm[:, 1:2], in0=lsm[:, 0:1], scalar1=-1.0, scalar2=None,
                                           op0=ALU.mult), r=[B_lsm], pw=[B_lsm])
        fw.op(act, lambda tt=tt: A.activation(out=aff_s[:, tt, :], in_=PS[6][:, 0:16], func=AF.Exp,
                                              bias=lsm[:, 1:2], accum_out=lsm[:, 2:3]),
              r=[BPS[6], B_lsm], pw=[B_affs, B_lsm])
        fw.op(dve, lambda: V.reciprocal(out=lsm[:, 3:4], in_=lsm[:, 2:3]), r=[B_lsm], pw=[B_lsm])
        fw.op(dve, lambda tt=tt: V.tensor_scalar(out=aff_s[:, tt, :], in0=aff_s[:, tt, :], scalar1=lsm[:, 3:4],
                                                 scalar2=None, op0=ALU.mult), r=[B_lsm], w=[B_affs])
    fw.dma(sp, affloc[:, :].rearrange("(a p) e -> p a e", p=128), aff_s[:], B_affs, r=[B_affs], pw=[B_affloc])
    fw.coll(x1bf[:, :], x1all[:, :], r=[B_x1bf], w=[B_x1all])
    fw.coll(affloc[:, :], affall[:, :], r=[B_affloc], w=[B_affall])
    fw.barrier()
    s3.close()
    sA.close()

    if stage <= 1:
        sd = contextlib.ExitStack()
        t_ = sbt(sd, "dbgt", [128, D], F32); Bt = Buf("dbgt")
        for tt in range(8):
            fw.dma(sp, t_[:], x1f[tt * 128:(tt + 1) * 128, :], Bt, r=[B_x1f], w=[Bt])
            fw.dma(sp, out_d[tt * 128:(tt + 1) * 128, :], t_[:], Bt, r=[Bt])
        fw.barrier()
        sd.close()
        top.close()
        return nc

    sB = contextlib.ExitStack()
    idxs = sbt(sB, "idxs", [128, 16], I32); B_idxs = Buf("idxs")
    gate = sbt(sB, "gate", [128, 16], F32); B_gate = Buf("gate")

    sb1 = contextlib.ExitStack()
    Ab = [sbt(sb1, f"Ab{b}", [128, 32, 16], F32) for b in range(2)]; B_Ab = [Buf("Ab0"), Buf("Ab1")]
    sel_s = sbt(sb1, "sel_s", [128, 2, 16], F32); B_sel = Buf("sel")
    tmpA = sbt(sb1, "tmpA", [128, 32, 16], F32); B_tmpA = Buf("tmpA")
    vg = sbt(sb1, "vg", [128, 4, 32], F32); B_vg = Buf("vg")
    lo = sbt(sb1, "lo", [128, 4], F32); hi = sbt(sb1, "hi", [128, 4], F32); mid = sbt(sb1, "mid", [128, 4], F32)
    cntp = sbt(sb1, "cntp", [128, 4], F32); ge = sbt(sb1, "ge", [128, 4], F32)
    t1 = sbt(sb1, "t1", [128, 4], F32)
    B_lo, B_hi, B_mid, B_cntp, B_ge, B_t1 = Buf("lo"), Buf("hi"), Buf("mid"), Buf("cntp"), Buf("ge"), Buf("t1")
    jk = sbt(sb1, "jk", [128, 32], F32); B_jk = Buf("jk")
    ones_s = sbt(sb1, "ones_s", [128, 128], F32); ltri_s = sbt(sb1, "ltri_s", [128, 128], F32)
    B_ones, B_ltri = Buf("ones"), Buf("ltri")
    iota_s = sbt(sb1, "iota_s", [128, 512], F32); B_iota = Buf("iota")
    tdec = sbt(sb1, "tdec_s", [128, 2, 32, 2], F32); B_tdec = Buf("tdec")
    coreoff = sbt(sb1, "coreoff_s", [128, 1], F32); B_coreoff = Buf("coreoff")
    maskg = sbt(sb1, "maskg", [128, 4, 32], F32); B_mask = Buf("mask")
    incl = sbt(sb1, "incl", [128, 4, 32], F32); B_incl = Buf("incl")
    posg = sbt(sb1, "posg", [128, 4, 32], F32); B_pos = Buf("pos")
    onesr = sbt(sb1, "onesr", [128, 32], F32); B_onesr = Buf("onesr")
    Rg = sbt(sb1, "Rg", [128, 4, 32, 4], F32); B_Rg = Buf("Rg")
    PMs = [sbt(sb1, f"PMs{b}", [128, 32, 4], F32) for b in range(2)]; B_PMs = [Buf("PMs0"), Buf("PMs1")]
    OH = [sbt(sb1, f"OH{i}", [128, 512], F32) for i in range(2)]; B_OH = [Buf("OH0"), Buf("OH1")]
    slf = sbt(sb1, "slf", [128, 4], F32); B_slf = Buf("slf")

    for b in range(2):
        fw.dma(sp, Ab[b][:].rearrange("p a e -> p (a e)"),
               affall[b * S:(b + 1) * S, :].rearrange("(p a) e -> p (a e)", p=128), B_Ab[b],
               r=[B_affall], w=[B_Ab[b]])
    fw.dma(sp, sel_s[:].rearrange("p a e -> p (a e)"), sel_d[:, :], B_sel, w=[B_sel])
    fw.dma(sp, ones_s[:], ones_d[:, :], B_ones, w=[B_ones])
    fw.dma(sp, ltri_s[:], ltri_d[:, :], B_ltri, w=[B_ltri])
    fw.dma(sp, iota_s[:], iota_d[:, :], B_iota, w=[B_iota])
    fw.dma(sp, tdec[:].rearrange("p a b c -> p (a b c)"), tdec_d[:, :], B_tdec, w=[B_tdec])
    fw.dma(sp, coreoff[:], coreoff_d[:, :], B_coreoff, w=[B_coreoff])
    fw.op(dve, lambda: V.memset(onesr[:], 1.0), w=[B_onesr])
    fw.op(dve, lambda: V.memset(Rg[:].rearrange("p a b c -> p (a b c)"), 0.0), w=[B_Rg])
    for g in range(4):
        el, b = g // 2, g % 2
        fw.op(dve, lambda el=el, b=b: V.tensor_tensor(
            out=tmpA[:], in0=Ab[b][:], in1=sel_s[:, el:el + 1, :].to_broadcast([128, 32, 16]), op=ALU.mult),
            r=[B_Ab[b], B_sel], w=[B_tmpA])
        fw.op(dve, lambda g=g: V.tensor_reduce(out=vg[:, g, :], in_=tmpA[:], axis=AX.X, op=ALU.add),
              r=[B_tmpA], pw=[B_vg])
    fw.op(dve, lambda: V.memset(lo[:], 0.0), w=[B_lo])
    fw.op(dve, lambda: V.memset(hi[:], 1.0), w=[B_hi])
    for it in range(NBIS):
        fw.op(dve, lambda: V.tensor_tensor(out=mid[:], in0=lo[:], in1=hi[:], op=ALU.add), r=[B_lo, B_hi], w=[B_mid])
        fw.op(dve, lambda: V.tensor_scalar(out=mid[:], in0=mid[:], scalar1=0.5, scalar2=None, op0=ALU.mult),
              w=[B_mid])
        for g in range(4):
            fw.op(dve, lambda g=g: V.tensor_scalar(out=jk[:], in0=vg[:, g, :], scalar1=mid[:, g:g + 1], scalar2=None,
                                                   op0=ALU.is_ge, op1=ALU.add, accum_out=cntp[:, g:g + 1]),
                  r=[B_vg, B_mid], w=[B_jk], pw=[B_cntp])
        fw.op(pe, lambda: T.matmul(PS[0][:, 0:4], ones_s[:], cntp[:], start=True, stop=True),
              r=[B_ones, B_cntp], w=[BPS[0]])
        fw.op(dve, lambda: V.tensor_scalar(out=ge[:], in0=PS[0][:, 0:4], scalar1=CAP - 0.5, scalar2=None,
                                           op0=ALU.is_ge), r=[BPS[0]], w=[B_ge])
        fw.op(dve, lambda: V.tensor_tensor(out=t1[:], in0=ge[:], in1=mid[:], op=ALU.mult), r=[B_ge, B_mid], w=[B_t1])
        fw.op(dve, lambda: V.tensor_tensor(out=lo[:], in0=lo[:], in1=t1[:], op=ALU.max), r=[B_t1], w=[B_lo])
        fw.op(dve, lambda: V.scalar_tensor_tensor(out=t1[:], in0=ge[:], scalar=2.0, in1=mid[:], op0=ALU.mult,
                                                  op1=ALU.add), r=[B_ge, B_mid], w=[B_t1])
        fw.op(dve, lambda: V.tensor_tensor(out=hi[:], in0=hi[:], in1=t1[:], op=ALU.min), r=[B_t1], w=[B_hi])
    for g in range(4):
        fw.op(dve, lambda g=g: V.tensor_scalar(out=maskg[:, g, :], in0=vg[:, g, :], scalar1=lo[:, g:g + 1],
                                               scalar2=None, op0=ALU.is_ge), r=[B_vg, B_lo], pw=[B_mask])
    for g in range(4):
        fw.op(dve, lambda g=g: V.tensor_tensor_scan(out=incl[:, g, :], data0=onesr[:], data1=maskg[:, g, :],
                                                    initial=0.0, op0=ALU.mult, op1=ALU.add),
              r=[B_onesr, B_mask], pw=[B_incl])
    fw.op(dve, lambda: V.tensor_copy(out=cntp[:], in_=incl[:, :, 31]), r=[B_incl], w=[B_cntp])
    fw.op(pe, lambda: T.matmul(PS[1][:, 0:4], ltri_s[:], cntp[:], start=True, stop=True),
          r=[B_ltri, B_cntp], w=[BPS[1]])
    fw.op(dve, lambda: V.tensor_copy(out=slf[:], in_=PS[1][:, 0:4]), r=[BPS[1]], w=[B_slf])
    for g in range(4):
        el, b = g // 2, g % 2
        fw.op(dve, lambda g=g: V.tensor_scalar(out=posg[:, g, :], in0=incl[:, g, :], scalar1=slf[:, g:g + 1],
                                               scalar2=None, op0=ALU.add), r=[B_incl, B_slf], pw=[B_pos])
        fw.op(dve, lambda g=g: V.tensor_tensor(out=posg[:, g, :], in0=posg[:, g, :], in1=maskg[:, g, :],
                                               op=ALU.subtract), r=[B_mask], w=[B_pos])
        fw.op(dve, lambda g=g, el=el, b=b: V.tensor_scalar(out=PMs[b][:, :, el], in0=posg[:, g, :],
                                                           scalar1=coreoff[:, 0:1], scalar2=float(g * 512),
                                                           op0=ALU.add, op1=ALU.add),
              r=[B_pos, B_coreoff], pw=[B_PMs[b]])
        fw.op(dve, lambda el=el, b=b: V.tensor_scalar(out=PMs[b][:, :, el], in0=PMs[b][:, :, el],
                                                      scalar1=float(NCORES * 2048 - 1), scalar2=None, op0=ALU.min),
              w=[B_PMs[b]])
        fw.op(dve, lambda g=g, el=el, b=b: V.tensor_copy(out=PMs[b][:, :, 2 + el], in_=maskg[:, g, :]),
              r=[B_mask], w=[B_PMs[b]])
        fw.op(dve, lambda g=g, b=b: V.tensor_copy(out=Rg[:, g, :, 0:2], in_=tdec[:, b, :, :]),
              r=[B_tdec], pw=[B_Rg])
        fw.op(dve, lambda g=g: V.tensor_copy(out=Rg[:, g, :, 2], in_=vg[:, g, :]), r=[B_vg], pw=[B_Rg])
    for b in range(2):
        fw.dma(sp, pmloc[b * S:(b + 1) * S, :].rearrange("(p a) e -> p (a e)", p=128),
               PMs[b][:].rearrange("p a e -> p (a e)"), B_PMs[b], r=[B_PMs[b]], pw=[B_pmloc])
    fw.coll(pmloc[:, :], pmall[:, :], r=[B_pmloc], w=[B_pmall])
    for g in range(4):
        for j in range(32):
            oh, Bo = OH[j % 2], B_OH[j % 2]
            fw.op(dve, lambda g=g, j=j, oh=oh: V.tensor_scalar(
                out=oh[:], in0=iota_s[:], scalar1=posg[:, g, j:j + 1], scalar2=maskg[:, g, j:j + 1],
                op0=ALU.is_equal, op1=ALU.mult), r=[B_iota, B_pos, B_mask], w=[Bo])
            for sc in range(4):
                fw.op(pe, lambda g=g, j=j, sc=sc, oh=oh: T.matmul(
                    PS[4 + sc][:, 0:4], oh[:, sc * 128:(sc + 1) * 128], Rg[:, g, j, :],
                    start=(j == 0), stop=(j == 31)),
                    r=[Bo, B_Rg], w=[BPS[4 + sc]] if j == 0 else (), pw=[BPS[4 + sc]] if j else (),
                    inc=(sc == 3))
        for sc in range(4):
            col = g * 4 + sc
            fw.op(dve, lambda sc=sc: V.tensor_copy(out=t1[:], in_=PS[4 + sc][:, 0:4]), r=[BPS[4 + sc]], w=[B_t1])
            fw.op(dve, lambda sc=sc: V.scalar_tensor_tensor(out=slf[:, 0:1], in0=t1[:, 0:1], scalar=64.0,
                                                            in1=t1[:, 1:2], op0=ALU.mult, op1=ALU.add),
                  r=[B_t1], w=[B_slf])
            fw.op(dve, lambda col=col: V.tensor_copy(out=idxs[:, col:col + 1], in_=slf[:, 0:1]),
                  r=[B_slf], pw=[B_idxs])
            fw.op(dve, lambda col=col, sc=sc: V.tensor_copy(out=gate[:, col:col + 1], in_=t1[:, 2:3]),
                  r=[B_t1], pw=[B_gate])
    fw.barrier()
    sb1.close()

    xinT = sbt(sB, "xinT", [128, 16, 1024], BF16); B_xin = Buf("xin")
    hT = sbt(sB, "hT", [128, NF, 1024], BF16); B_hT = [Buf(f"hT{f}") for f in range(NF)]
    xg = [sbt(sB, f"xg{i}", [128, D], BF16) for i in range(2)]; B_xg = [Buf("xg0"), Buf("xg1")]
    NGU = 3
    wgu = [sbt(sB, f"wgu{i}", [128, 2, 16, 128], BF16) for i in range(NGU)]; B_wgu = [Buf(f"wgu{i}") for i in range(NGU)]
    NWD = 6
    wdn = [sbt(sB, f"wdn{i}", [128, 512], BF16) for i in range(NWD)]; B_wdn = [Buf(f"wdn{i}") for i in range(NWD)]
    sg = [sbt(sB, f"sg{i}", [128, 512], F32) for i in range(2)]; B_sg = [Buf("sg0"), Buf("sg1")]
    yst = [sbt(sB, f"yst{i}", [128, 512], F32) for i in range(4)]; B_yst = [Buf(f"yst{i}") for i in range(4)]

    icolB = [sbt(sB, f"icolB{i}", [128, 1], I32) for i in range(4)]; B_icolB = [Buf(f"icolB{i}") for i in range(4)]
    icb_i = [0]
    gu_i = [0]
    wd_i = [0]
    yst_i = [0]
    for el in range(2):
        for b in range(2):
            g = el * 2 + b
            for sc in range(4):
                col = g * 4 + sc
                xq, Bq = xg[sc % 2], B_xg[sc % 2]
                ii = icb_i[0] % 4; icb_i[0] += 1
                fw.op(dve, lambda ii=ii, col=col: V.tensor_copy(out=icolB[ii][:], in_=idxs[:, col:col + 1]),
                      r=[B_idxs], w=[B_icolB[ii]])
                harden(icolB[ii], B_icolB[ii])
                fw.dma(pool, xq[:], x1all[:, :], Bq, r=[B_x1all, B_icolB[ii]], w=[Bq], indirect=icolB[ii][:, :])
                s0 = b * 512 + sc * 128
                for k4 in range(4):
                    tb = 6 + (k4 % 2)
                    for j in range(4):
                        k = k4 * 4 + j
                        fw.op(pe, lambda tb=tb, j=j, k=k, xq=xq: T.transpose(
                            PS[tb][:].bitcast(BF16)[:, j * 128:(j + 1) * 128], xq[:, k * 128:(k + 1) * 128],
                            ident_b[:]),
                            r=[Bq, B_identb], w=[BPS[tb]] if j == 0 else (), pw=[BPS[tb]] if j else (),
                            inc=(j == 3))
                    fw.op(act, lambda tb=tb, k4=k4, s0=s0: A.copy(
                        out=xinT[:, k4 * 4:(k4 + 1) * 4, s0:s0 + 128],
                        in_=PS[tb][:].bitcast(BF16)[:, 0:512].rearrange("p (a b) -> p a b", a=4)),
                        r=[BPS[tb]], pw=[B_xin])
        def load_gu(f):
            i = gu_i[0]; gu_i[0] += 1
            s = i % NGU
            fw.dma(pool, wgu[s][:].rearrange("p a k j -> p (a k j)"), wgu_r[el, f], B_wgu[s], w=[B_wgu[s]])
            return s
        slots = {}
        slots[0] = load_gu(0)
        slots[1] = load_gu(1)
        for f in range(NF):
            if f + 2 < NF:
                slots[f + 2] = load_gu(f + 2)
            s = slots[f]
            for half in range(2):
                bg = (f * 2 + half) % 2 * 2
                for gu in range(2):
                    for k in range(16):
                        fw.op(pe, lambda gu=gu, k=k, s=s, half=half, bg=bg: T.matmul(
                            PS[bg + gu][:, :], wgu[s][:, gu, k, :], xinT[:, k, half * 512:(half + 1) * 512],
                            start=(k == 0), stop=(k == 15)),
                            r=[B_wgu[s], B_xin], w=[BPS[bg + gu]] if k == 0 else (),
                            pw=[BPS[bg + gu]] if k else (), inc=(k == 15))
                sgi = (f * 2 + half) % 2
                fw.op(act, lambda bg=bg, sgi=sgi: A.activation(out=sg[sgi][:], in_=PS[bg][:, :], func=AF.Silu),
                      r=[BPS[bg]], w=[B_sg[sgi]])
                fw.op(dve, lambda bg=bg, sgi=sgi, f=f, half=half: V.tensor_tensor(
                    out=hT[:, f, half * 512:(half + 1) * 512], in0=PS[bg + 1][:, :], in1=sg[sgi][:], op=ALU.mult),
                    r=[BPS[bg + 1], B_sg[sgi]], pw=[B_hT[f]])
        def load_wd(q, f):
            i = wd_i[0]; wd_i[0] += 1
            s = i % NWD
            fw.dma(pool, wdn[s][:], wd_r[el, q, f], B_wdn[s], w=[B_wdn[s]])
            return s
        seq = [(q, f) for q in range(4) for f in range(NF)]
        dsl = {}
        PRE = NWD - 1
        for i in range(PRE):
            dsl[seq[i]] = load_wd(*seq[i])
        for i, (q, f) in enumerate(seq):
            if i + PRE < len(seq):
                dsl[seq[i + PRE]] = load_wd(*seq[i + PRE])
            s = dsl[(q, f)]
            for st in range(8):
                fw.op(pe, lambda st=st, s=s, f=f: T.matmul(
                    PS[st][:, :], hT[:, f, st * 128:(st + 1) * 128], wdn[s][:], start=(f == 0), stop=(f == NF - 1)),
                    r=[B_hT[f], B_wdn[s]], w=[BPS[st]] if f == 0 else (), pw=[BPS[st]] if f else (),
                    inc=(st == 7 or f == NF - 1))
            if f == NF - 1:
                for st in range(8):
                    b, sc = st // 4, st % 4
                    col = (el * 2 + b) * 4 + sc
                    yi = yst_i[0] % 4; yst_i[0] += 1
                    eng, EE = (dve, V) if st % 2 == 0 else (act, A)
                    if st % 2 == 0:
                        fw.op(dve, lambda st=st, yi=yi, col=col: V.tensor_scalar(
                            out=yst[yi][:], in0=PS[st][:, :], scalar1=gate[:, col:col + 1], scalar2=None,
                            op0=ALU.mult), r=[BPS[st], B_gate], w=[B_yst[yi]])
                    else:
                        fw.op(act, lambda st=st, yi=yi, col=col: A.activation(
                            out=yst[yi][:], in_=PS[st][:, :], func=AF.Identity, scale=gate[:, col:col + 1]),
                            r=[BPS[st], B_gate], w=[B_yst[yi]])
                    row0 = el * 1024 + st * 128
                    fw.dma(sp, yloc[row0:row0 + 128, q * 512:(q + 1) * 512], yst[yi][:], B_yst[yi],
                           r=[B_yst[yi]], pw=[B_yloc])
    fw.coll(yloc[:, :], yall[:, :], r=[B_yloc], w=[B_yall])
    fw.barrier()
    sB.close()

    sC = contextlib.ExitStack()
    g2 = sbt(sC, "g2", [128, D], F32); b2 = sbt(sC, "b2", [128, D], F32); B_ln2 = Buf("ln2")
    ipm_t = [sbt(sC, f"ipm{c}", [128, 1], I32) for c in range(64)]; B_idxPM = Buf("idxPM")
    pm = [sbt(sC, f"pm{i}", [128, 8, 4], F32) for i in range(2)]; B_pm = [Buf("pm0"), Buf("pm1")]
    pmi = [sbt(sC, f"pmi{i}", [128, 8, 2], I32) for i in range(2)]; B_pmi = [Buf("pmi0"), Buf("pmi1")]
    acc_t = [sbt(sC, f"acc{i}", [128, D], F32) for i in range(2)]; B_acc = [Buf("acc0"), Buf("acc1")]
    xo = [sbt(sC, f"xo{i}", [128, D], F32) for i in range(2)]; B_xo = [Buf("xo0"), Buf("xo1")]
    NG = 4
    G = [sbt(sC, f"G{i}", [128, D], F32) for i in range(NG)]; B_G = [Buf(f"G{i}") for i in range(NG)]
    stats = sbt(sC, "stats2", [128, 4, 6], F32); B_stats = Buf("stats2")
    mv = sbt(sC, "mv2", [128, 8], F32); B_mv = Buf("mv2")
    fw.dma(sp, g2[:], ln2g[:, :], B_ln2, pw=[B_ln2])
    fw.dma(sp, b2[:], ln2b[:, :], B_ln2, pw=[B_ln2])
    for c in range(64):
        fw.dma(sp, ipm_t[c][:], idxPM_d[c], B_idxPM, pw=[B_idxPM])
    gi = [0]
    icolC = [sbt(sC, f"icolC{i}", [128, 1], I32) for i in range(6)]; B_icolC = [Buf(f"icolC{i}") for i in range(6)]
    icc_i = [0]
    for tt in range(8):
        p_, Bp_ = pm[tt % 2], B_pm[tt % 2]
        pi_, Bpi_ = pmi[tt % 2], B_pmi[tt % 2]
        ac, Bac = acc_t[tt % 2], B_acc[tt % 2]
        xo_, Bxo_ = xo[tt % 2], B_xo[tt % 2]
        for c8 in range(8):
            fw.dma(pool, p_[:, c8, :], pmall[:, :], Bp_, r=[B_pmall, B_idxPM], pw=[Bp_],
                   indirect=ipm_t[tt * 8 + c8][:, :])
        fw.op(dve, lambda p_=p_, pi_=pi_: V.tensor_copy(out=pi_[:], in_=p_[:, :, 0:2]), r=[Bp_], w=[Bpi_])
        fw.dma(sp, xo_[:], x1f[tt * 128:(tt + 1) * 128, :], Bxo_, r=[B_x1f], w=[Bxo_])
        fw.op(dve, lambda ac=ac, xo_=xo_: V.tensor_scalar(out=ac[:], in0=xo_[:], scalar1=ALPHA, scalar2=None,
                                                         op0=ALU.mult), r=[Bxo_], w=[Bac])
        for c8 in range(8):
            for el in range(2):
                i = gi[0] % NG; gi[0] += 1
                ii = icc_i[0] % 6; icc_i[0] += 1
                fw.op(dve, lambda ii=ii, pi_=pi_, c8=c8, el=el: V.tensor_copy(out=icolC[ii][:], in_=pi_[:, c8, el:el + 1]),
                      r=[Bpi_], w=[B_icolC[ii]])
                harden(icolC[ii], B_icolC[ii])
                fw.dma(pool, G[i][:], yall[:, :], B_G[i], r=[B_yall, B_icolC[ii]], w=[B_G[i]],
                       indirect=icolC[ii][:, :])
                fw.op(dve, lambda i=i, ac=ac, p_=p_, c8=c8, el=el: V.scalar_tensor_tensor(
                    out=ac[:], in0=G[i][:], scalar=p_[:, c8, 2 + el:3 + el], in1=ac[:], op0=ALU.mult, op1=ALU.add),
                    r=[B_G[i], Bp_], w=[Bac])

        def ln2(r_t, B_r, out_t, B_out):
            for c4 in range(4):
                fw.op(dve, lambda c4=c4: V.bn_stats(out=stats[:, c4, :], in_=r_t[:, c4 * 512:(c4 + 1) * 512]),
                      r=[B_r], pw=[B_stats])
            fw.op(dve, lambda: V.bn_aggr(out=mv[:, 0:2], in_=stats[:].rearrange("p a b -> p (a b)")),
                  r=[B_stats], pw=[B_mv])
            fw.op(dve, lambda: V.tensor_scalar(out=mv[:, 2:3], in0=mv[:, 1:2], scalar1=LN_EPS, scalar2=None,
                                               op0=ALU.add), r=[B_mv], pw=[B_mv])
            fw.op(act, lambda: A.sqrt(out=mv[:, 3:4], in_=mv[:, 2:3]), r=[B_mv], pw=[B_mv])
            fw.op(dve, lambda: V.reciprocal(out=mv[:, 4:5], in_=mv[:, 3:4]), r=[B_mv], pw=[B_mv])
            fw.op(dve, lambda: V.tensor_scalar(out=r_t[:], in0=r_t[:], scalar1=mv[:, 0:1], scalar2=mv[:, 4:5],
                                               op0=ALU.subtract, op1=ALU.mult), r=[B_mv], w=[B_r])
            fw.op(dve, lambda: V.tensor_tensor(out=r_t[:], in0=r_t[:], in1=g2[:], op=ALU.mult),
                  r=[B_ln2], w=[B_r])
            fw.op(dve, lambda: V.tensor_tensor(out=out_t[:], in0=r_t[:], in1=b2[:], op=ALU.add),
                  r=[B_ln2, B_r], w=[B_out])
        ln2(ac, Bac, xo_, Bxo_)
        fw.dma(sp, out_d[tt * 128:(tt + 1) * 128, :], xo_[:], Bxo_, r=[Bxo_])
    fw.barrier()
    sC.close()
    top.close()
    return nc


def _consts():
    c = {}
    Rm = np.zeros((128, 128), np.float32)
    for m in range(128):
        if (m % 64) < 32:
            Rm[m + 32, m] = -1.0
        else:
            Rm[m - 32, m] = 1.0
    c["Rm"] = Rm
    c["ident"] = np.eye(128, dtype=np.float32)
    c["ltri"] = np.triu(np.ones((128, 128), np.float32), 1)
    c["ones"] = np.ones((128, 128), np.float32)
    c["iota512"] = np.tile(np.arange(512, dtype=np.float32)[None, :], (128, 1))
    td = np.zeros((128, 2, 32, 2), np.float32)
    for b in range(2):
        t = b * S + np.arange(128)[:, None] * 32 + np.arange(32)[None, :]
        td[:, b, :, 0] = t // 64
        td[:, b, :, 1] = t % 64
    c["tdec"] = td.reshape(128, -1)
    return c


def _rope_tables(pos):
    inv = (10000.0 ** (-np.arange(0, 64, 2, dtype=np.float32) / np.float32(64))).astype(np.float32)
    ang = pos.astype(np.float32)[:, None] * inv[None, :]
    ang = np.concatenate([ang, ang], axis=-1)
    cos = np.cos(ang).astype(np.float32); sin = np.sin(ang).astype(np.float32)
    cosT = np.concatenate([cos.T, cos.T], axis=0)
    sinT = np.concatenate([sin.T, sin.T], axis=0)
    return np.ascontiguousarray(cosT), np.ascontiguousarray(sinT)


def _invcnt(t0):
    out = np.zeros((4, TOK), np.float32)
    t = t0 + np.arange(TOK)
    for gi, w in enumerate((2, 4, 8, 16)):
        lo = np.clip(t - w // 2, 0, S); hi = np.clip(t + w - w // 2, 0, S)
        out[gi] = 1.0 / (hi - lo).astype(np.float32)
    return np.tile(out.reshape(1, -1), (128, 1))


_PROG = {}


def make_inputs(x, w_in, pool_w, pool_scale, lambda_q1, lambda_k1, lambda_q2, lambda_k2, subln_g, w_out,
                ln1_g, ln1_b, w_router, w_gate, w_up, w_down, ln2_g, ln2_b):
    f = np.float32
    x = np.asarray(x, f); w_in = np.asarray(w_in, f)[0]
    cst = _consts()
    rep = lambda v: np.ascontiguousarray(np.tile(np.asarray(v, f).reshape(1, -1), (128, 1)))
    shared = dict(cst)
    shared["w_in_r"] = np.ascontiguousarray(
        w_in.reshape(16, 128, 32, 128).transpose(2, 1, 0, 3).reshape(32, 128, 2048))
    pw = np.asarray(pool_w, f)[0]
    shared["poolw_r"] = np.ascontiguousarray(pw.reshape(4, 2, 128, 256).transpose(2, 0, 1, 3).reshape(128, 2048))
    shared["pscale"] = np.ascontiguousarray(np.asarray(pool_scale, f)[0].reshape(8, 128).T)
    shared["lam_in"] = rep(np.concatenate([np.asarray(lambda_q1, f)[0], np.asarray(lambda_k1, f)[0],
                                           np.asarray(lambda_q2, f)[0], np.asarray(lambda_k2, f)[0]]))
    shared["subg"] = rep(np.asarray(subln_g, f)[0])
    shared["subgc"] = np.ascontiguousarray(np.asarray(subln_g, f)[0].reshape(128, 1))
    shared["w_out_r"] = np.ascontiguousarray(
        np.asarray(w_out, f)[0].reshape(16, 128, D).transpose(1, 0, 2).reshape(128, 16 * D))
    shared["ln1g"] = rep(np.asarray(ln1_g, f)[0]); shared["ln1b"] = rep(np.asarray(ln1_b, f)[0])
    shared["ln2g"] = rep(np.asarray(ln2_g, f)[0]); shared["ln2b"] = rep(np.asarray(ln2_b, f)[0])
    shared["wr_r"] = np.ascontiguousarray(
        np.asarray(w_router, f)[0].reshape(16, 128, 16).transpose(1, 0, 2).reshape(128, 256))
    wg = np.asarray(w_gate, f)[0]; wu = np.asarray(w_up, f)[0]; wd = np.asarray(w_down, f)[0]
    in_maps = []
    for c in range(NCORES):
        b, j = c // 4, c % 4
        t0 = j * TOK
        m = dict(shared)
        xp = np.zeros((TOK + 16, D), f)
        lo, hi = t0 - 8, t0 + TOK + 8
        slo, shi = max(lo, 0), min(hi, S)
        xp[slo - lo:shi - lo] = x[b, slo:shi]
        m["xT"] = np.ascontiguousarray(xp.T).reshape(16, 128, TOK + 16)
        m["xtok"] = np.ascontiguousarray(x[b, t0:t0 + TOK])
        m["cosT"], m["sinT"] = _rope_tables(np.arange(t0, t0 + TOK))
        m["invcnt"] = _invcnt(t0)
        idx = np.zeros((128, 64), np.int32)
        p = np.arange(128)
        for h in range(8):
            for r in range(4):
                idx[:, h * 4 + r] = (4 * b + r) * 2048 + h * 128 + p
                idx[:, 32 + h * 4 + r] = (4 * b + r) * 2048 + 1024 + h * 128 + p
        m["idxKV"] = np.ascontiguousarray(idx.T).reshape(64, 128, 1)
        sel = np.zeros((128, 2, 16), f)
        sel[:, 0, 2 * c] = 1.0; sel[:, 1, 2 * c + 1] = 1.0
        m["sel"] = sel.reshape(128, 32)
        m["coreoff"] = np.full((128, 1), c * 2048, f)
        ipm = np.zeros((128, 64), np.int32)
        for tt in range(8):
            for c8 in range(8):
                ipm[:, tt * 8 + c8] = c8 * 2 * S + c * TOK + tt * 128 + p
        m["idxPM"] = np.ascontiguousarray(ipm.T).reshape(64, 128, 1)
        wgu = np.empty((2, NF, 128, 2, 16, 128), f)
        wdr = np.empty((2, 4, NF, 128, 512), f)
        for el in range(2):
            e = 2 * c + el
            wgu[el, :, :, 0] = wg[e].reshape(16, 128, NF, 128).transpose(2, 1, 0, 3)
            wgu[el, :, :, 1] = wu[e].reshape(16, 128, NF, 128).transpose(2, 1, 0, 3)
            wdr[el] = wd[e].reshape(NF, 128, 4, 512).transpose(2, 0, 1, 3)
        m["wgu_r"] = wgu.reshape(2, NF, 128, 4096)
        m["wd_r"] = wdr
        in_maps.append(m)
    return in_maps


def kernel(**inputs):
    in_maps = make_inputs(**inputs)
    if "nc" not in _PROG:
        _PROG["nc"] = build_program()
    res = run_bass_kernel_spmd(_PROG["nc"], in_maps, core_ids=list(range(NCORES)))
    out = np.concatenate([np.asarray(res.results[c]["out"], np.float32) for c in range(NCORES)], axis=0)
    return out.reshape(2, S, D)
```

```python
import contextlib
import numpy as np
import ml_dtypes
import concourse.bass as bass
import concourse.mybir as mybir
from concourse.bass_utils import run_bass_kernel_spmd

F32 = mybir.dt.float32
BF16 = mybir.dt.bfloat16
I32 = mybir.dt.int32
AF = mybir.ActivationFunctionType
ALU = mybir.AluOpType
AX = mybir.AxisListType

NCORES = 8
D = 2048
S = 4096
TOK = 1024
NF = 43
DFF = 5504
CAP = 512
ALPHA = 2.0 ** 0.25
LAM_INIT = 0.8 - 0.6 * 1.0
LN_EPS = 1e-5
NBIS = 32


class Buf:
    def __init__(self, name):
        self.name = name
        self.base = {}
        self.part = {}
        self.reads = {}
        self.dsem = None
        self.dval = 0


def _merge(dst, src):
    for k, (s, v) in src.items():
        if k not in dst or dst[k][1] < v:
            dst[k] = (s, v)


class Eng:
    def __init__(self, fw, eng, name, is_pe=False):
        self.fw = fw
        self.e = eng
        self.name = name
        self.sem = fw.newsem("s_" + name)
        self.n = 0
        self.waited = {}
        self.is_pe = is_pe
        self.pend = []

    def wait_for(self, deps):
        for k, (s, v) in deps.items():
            if k is self and self.is_pe:
                continue
            if v > self.waited.get(k, 0):
                self.e.wait_ge(s, v)
                self.waited[k] = v


class FW:
    def __init__(self, nc, stack):
        self.nc = nc
        self.stack = stack
        self.nsem = 0
        self.pe = Eng(self, nc.tensor, "pe", is_pe=True)
        self.act = Eng(self, nc.scalar, "act")
        self.dve = Eng(self, nc.vector, "dve")
        self.pool = Eng(self, nc.gpsimd, "pool")
        self.sp = Eng(self, nc.sync, "sp")
        self.engs = [self.pe, self.act, self.dve, self.pool, self.sp]
        self.dbufs = []

    def newsem(self, name):
        self.nsem += 1
        return self.stack.enter_context(self.nc.semaphore(f"{name}_{self.nsem}"))

    def _collect(self, r, w, pw):
        deps = {}
        for b in r:
            _merge(deps, b.base)
            _merge(deps, b.part)
        for b in w:
            _merge(deps, b.base)
            _merge(deps, b.part)
            _merge(deps, b.reads)
        for b in pw:
            if b.reads:
                nb = {}
                _merge(nb, b.base)
                _merge(nb, b.part)
                _merge(nb, b.reads)
                b.base, b.part, b.reads = nb, {}, {}
            _merge(deps, b.base)
        return deps

    def _record(self, tok, r, w, pw):
        k, s, v = tok
        for b in r:
            _merge(b.reads, {k: (s, v)})
        for b in w:
            b.base, b.part, b.reads = {k: (s, v)}, {}, {}
        for b in pw:
            _merge(b.part, {k: (s, v)})

    def op(self, E, fn, r=(), w=(), pw=(), inc=True):
        deps = self._collect(r, w, pw)
        E.wait_for(deps)
        ins = fn()
        if not inc:
            E.pend.append((tuple(r), tuple(w), tuple(pw)))
            return
        E.n += 1
        ins.then_inc(E.sem, 1)
        tok = (E, E.sem, E.n)
        self._record(tok, r, w, pw)
        for (pr, pw_, ppw) in E.pend:
            self._record(tok, pr, pw_, ppw)
        E.pend = []

    def dma(self, E, out, in_, sbuf, r=(), w=(), pw=(), indirect=None, **kw):
        deps = self._collect(r, w, pw)
        E.wait_for(deps)
        if sbuf.dsem is None:
            sbuf.dsem = self.newsem("d_" + sbuf.name)
            self.dbufs.append(sbuf)
        sbuf.dval += 16
        if indirect is None:
            E.e.dma_start(out=out, in_=in_, **kw).then_inc(sbuf.dsem, 16)
        else:
            E.e.indirect_dma_start(out=out, out_offset=None, in_=in_,
                                   in_offset=bass.IndirectOffsetOnAxis(ap=indirect, axis=0)).then_inc(sbuf.dsem, 16)
        tok = (("d", id(sbuf)), sbuf.dsem, sbuf.dval)
        self._record(tok, r, w, pw)

    def coll(self, in_ap, out_ap, r, w):
        E = self.pool
        deps = self._collect(r, w, ())
        E.wait_for(deps)
        sem = self.newsem("cc")
        E.e.collective_compute("AllGather", ALU.bypass, replica_groups=[list(range(NCORES))],
                               ins=[in_ap], outs=[out_ap]).then_inc(sem)
        tok = (("c", id(sem)), sem, 1)
        self._record(tok, r, w, ())

    def barrier(self):
        alld = {}
        for E in self.engs:
            if E.pend:
                raise RuntimeError("pending unsignalled ops on " + E.name)
            if E.n > 0:
                alld[E] = (E.sem, E.n)
        for b in self.dbufs:
            alld[("d", id(b))] = (b.dsem, b.dval)
        for E in self.engs:
            d = {k: v for k, v in alld.items() if k is not E}
            if E.n > 0 and not E.is_pe:
                d[E] = (E.sem, E.n)
            E.wait_for(d)


def build_program(stage=99, dbg=False):
    nc = bass.Bass("TRN2", target_bir_lowering=False)
    top = contextlib.ExitStack()
    fw = FW(nc, top)
    pe, act, dve, pool, sp = fw.pe, fw.act, fw.dve, fw.pool, fw.sp
    V, A, P, T = nc.vector, nc.scalar, nc.gpsimd, nc.tensor

    def din(name, shape, dt=F32):
        return nc.dram_tensor(name, shape, dt, kind="ExternalInput")

    xT = din("xT", [16, 128, 1040])
    xtok = din("xtok", [TOK, D])
    w_in_r = din("w_in_r", [32, 128, 2048])
    poolw_r = din("poolw_r", [128, 8 * 256])
    pscale = din("pscale", [128, 8])
    lam_in = din("lam_in", [128, 256])
    subg = din("subg", [128, 128])
    subgc_d = din("subgc", [128, 1])
    w_out_r = din("w_out_r", [128, 16 * 2048])
    ln1g = din("ln1g", [128, D]); ln1b = din("ln1b", [128, D])
    ln2g = din("ln2g", [128, D]); ln2b = din("ln2b", [128, D])
    wr_r = din("wr_r", [128, 256])
    cosT_d = din("cosT", [128, TOK]); sinT_d = din("sinT", [128, TOK])
    invcnt_d = din("invcnt", [128, 4 * TOK])
    Rm_d = din("Rm", [128, 128]); ident_d = din("ident", [128, 128])
    ltri_d = din("ltri", [128, 128]); ones_d = din("ones", [128, 128])
    iota_d = din("iota512", [128, 512])
    tdec_d = din("tdec", [128, 2 * 32 * 2])
    idxKV_d = din("idxKV", [64, 128, 1], I32)
    sel_d = din("sel", [128, 32])
    coreoff_d = din("coreoff", [128, 1])
    idxPM_d = din("idxPM", [64, 128, 1], I32)
    if stage >= 2:
        wgu_r = din("wgu_r", [2, NF, 128, 4096])
        wd_r = din("wd_r", [2, 4, NF, 128, 512])
    out_d = nc.dram_tensor("out", [TOK, D], F32, kind="ExternalOutput")
    dbg_d = nc.dram_tensor("dbg", [TOK, D], F32, kind="ExternalOutput") if dbg else None

    kvloc = nc.dram_tensor("kvloc", [2048, 1024], BF16)
    kvall = nc.dram_tensor("kvall", [NCORES * 2048, 1024], BF16)
    x1bf = nc.dram_tensor("x1bf", [TOK, D], BF16)
    x1all = nc.dram_tensor("x1all", [NCORES * TOK, D], BF16)
    x1f = nc.dram_tensor("x1f", [TOK, D], F32)
    affloc = nc.dram_tensor("affloc", [TOK, 16], F32)
    affall = nc.dram_tensor("affall", [NCORES * TOK, 16], F32)
    yloc = nc.dram_tensor("yloc", [2048, D], F32)
    yall = nc.dram_tensor("yall", [NCORES * 2048, D], F32)
    pmloc = nc.dram_tensor("pmloc", [2 * S, 4], F32)
    pmall = nc.dram_tensor("pmall", [NCORES * 2 * S, 4], F32)
    B_kvloc, B_kvall = Buf("kvloc"), Buf("kvall")
    B_x1bf, B_x1all, B_x1f = Buf("x1bf"), Buf("x1all"), Buf("x1f")
    B_affloc, B_affall = Buf("affloc"), Buf("affall")
    B_yloc, B_yall, B_pmloc, B_pmall = Buf("yloc"), Buf("yall"), Buf("pmloc"), Buf("pmall")

    def sbt(stack, name, shape, dt):
        return stack.enter_context(nc.sbuf_tensor(name, shape, dt))

    def early_exit(stacks):
        fw.barrier()
        sd = contextlib.ExitStack()
        t_ = sbt(sd, "dbgt", [128, D], F32); Bt = Buf("dbgt")
        for tt in range(8):
            fw.dma(sp, t_[:], xtok[tt * 128:(tt + 1) * 128, :], Bt, w=[Bt])
            fw.dma(sp, out_d[tt * 128:(tt + 1) * 128, :], t_[:], Bt, r=[Bt])
        fw.barrier()
        sd.close()
        for st_ in stacks:
            st_.close()
        top.close()
        return nc

    PS = [top.enter_context(nc.psum_tensor(f"ps{i}", [128, 512], F32)) for i in range(8)]
    BPS = [Buf(f"ps{i}") for i in range(8)]

    if stage == 0:
        sd = contextlib.ExitStack()
        t_ = sbt(sd, "dbgt", [128, D], F32); Bt = Buf("dbgt")
        for tt in range(8):
            fw.dma(sp, t_[:], xtok[tt * 128:(tt + 1) * 128, :], Bt, w=[Bt])
            fw.dma(sp, out_d[tt * 128:(tt + 1) * 128, :], t_[:], Bt, r=[Bt])
        fw.barrier()
        sd.close()
        top.close()
        return nc
    ident_f = sbt(top, "ident_f", [128, 128], F32); B_identf = Buf("identf")
    ident_b = sbt(top, "ident_b", [128, 128], BF16); B_identb = Buf("identb")
    dmy = sbt(top, "dmy", [128, 1], I32)

    def harden(tile, B):
        fw.op(dve, lambda: V.tensor_copy(out=dmy[:], in_=tile[:]), w=[B])
    fw.dma(sp, ident_f[:], ident_d[:, :], B_identf, w=[B_identf])
    fw.dma(pool, ident_b[:], ident_d[:, :], B_identb, w=[B_identb])

    sA = contextlib.ExitStack()
    mixedT = sbt(sA, "mixedT", [128, 16, TOK], BF16)
    B_mixed = [Buf(f"mixed{i}") for i in range(16)]
    qT = sbt(sA, "qT", [128, 8, TOK], BF16)
    B_q = [Buf(f"q{i}") for i in range(8)]

    s1 = contextlib.ExitStack()
    xT_s = sbt(s1, "xT_s", [128, 16, 1040], BF16); B_xT = Buf("xT")
    wring = [sbt(s1, f"wring{i}", [128, 16, 128], BF16) for i in range(4)]
    B_wr = [Buf(f"wring{i}") for i in range(4)]
    cosT = sbt(s1, "cosT_s", [128, TOK], F32); sinT = sbt(s1, "sinT_s", [128, TOK], F32)
    B_cs = Buf("cossin")
    Rm_b = sbt(s1, "Rm_b", [128, 128], BF16); B_Rm = Buf("Rm")
    invcnt = sbt(s1, "invcnt_s", [128, 4, TOK], F32); B_inv = Buf("invcnt")
    poolw = sbt(s1, "poolw_s", [128, 8, 256], BF16); B_poolw = Buf("poolw")
    pscale_s = sbt(s1, "pscale_s", [128, 8], F32); B_pscale = Buf("pscale")
    pooledT = sbt(s1, "pooledT", [128, 8, TOK], BF16); B_pooled = [Buf(f"pooled{i}") for i in range(8)]
    upool = sbt(s1, "upool", [128, 1040], F32); B_upool = Buf("upool")
    wsa = sbt(s1, "wsa", [128, 1040], F32); wsb = sbt(s1, "wsb", [128, 1040], F32)
    B_wsa, B_wsb = Buf("wsa"), Buf("wsb")
    ropea = [sbt(s1, f"ropea{i}", [128, 512], BF16) for i in range(2)]
    ropeb = [sbt(s1, f"ropeb{i}", [128, 512], BF16) for i in range(2)]
    B_ropea = [Buf("ropea0"), Buf("ropea1")]; B_ropeb = [Buf("ropeb0"), Buf("ropeb1")]
    kst = [sbt(s1, f"kst{i}", [128, TOK], BF16) for i in range(2)]; B_kst = [Buf("kst0"), Buf("kst1")]
    vst = [sbt(s1, f"vst{i}", [128, 8, 128], BF16) for i in range(2)]; B_vst = [Buf("vst0"), Buf("vst1")]

    for k in range(16):
        fw.dma(pool, xT_s[:, k, :], xT[k], B_xT, pw=[B_xT])
    fw.dma(sp, cosT[:], cosT_d[:, :], B_cs, pw=[B_cs])
    fw.dma(sp, sinT[:], sinT_d[:, :], B_cs, pw=[B_cs])
    fw.dma(pool, Rm_b[:], Rm_d[:, :], B_Rm, w=[B_Rm])
    fw.dma(sp, invcnt[:].rearrange("p g t -> p (g t)"), invcnt_d[:, :], B_inv, w=[B_inv])
    fw.dma(pool, poolw[:].rearrange("p a b -> p (a b)"), poolw_r[:, :], B_poolw, w=[B_poolw])
    fw.dma(sp, pscale_s[:], pscale[:, :], B_pscale, w=[B_pscale])

    ORDER = list(range(16, 32)) + list(range(0, 16))
    slot_of = {cc: pos % 4 for pos, cc in enumerate(ORDER)}

    def load_w(pos):
        cc = ORDER[pos]
        s = slot_of[cc]
        fw.dma(pool, wring[s][:].rearrange("p k j -> p (k j)"), w_in_r[cc], B_wr[s], w=[B_wr[s]])

    bank_rr = [0]

    def nextbank(lo=0, hi=4):
        b = lo + bank_rr[0] % (hi - lo)
        bank_rr[0] += 1
        return b

    def proj_T(cc, c0, n, bank):
        s = slot_of[cc]
        for k in range(16):
            fw.op(pe, lambda k=k: T.matmul(PS[bank][:, 0:n], wring[s][:, k, :], xT_s[:, k, c0:c0 + n],
                                            start=(k == 0), stop=(k == 15)),
                  r=[B_wr[s], B_xT], w=[BPS[bank]] if k == 0 else (), pw=[BPS[bank]] if k else (),
                  inc=(k == 15))

    load_w(0); load_w(1); load_w(2)
    WIN = (2, 4, 8, 16)
    for pos, cc in enumerate(ORDER):
        if pos + 3 < 32:
            load_w(pos + 3)
        if cc < 8:
            g = cc // 2
            wdw = WIN[g]
            for (c0, n) in ((0, 512), (512, 512), (1024, 16)):
                bk = nextbank()
                proj_T(cc, c0, n, bk)
                fw.op(act, lambda bk=bk, c0=c0, n=n: A.copy(out=upool[:, c0:c0 + n], in_=PS[bk][:, 0:n]),
                      r=[BPS[bk]], pw=[B_upool])
            cur, Bcur = upool, B_upool
            step = 1
            L = 1040
            pp = [(wsa, B_wsa), (wsb, B_wsb)]
            i = 0
            while step < wdw:
                nxt, Bn = pp[i % 2]
                L2 = L - step
                fw.op(dve, lambda cur=cur, nxt=nxt, L2=L2, step=step: V.tensor_tensor(
                    out=nxt[:, 0:L2], in0=cur[:, 0:L2], in1=cur[:, step:step + L2], op=ALU.add),
                    r=[Bcur], w=[Bn])
                cur, Bcur = nxt, Bn
                L = L2
                step *= 2
                i += 1
            o = 8 - wdw // 2
            nxt, Bn = pp[i % 2]
            fw.op(dve, lambda cur=cur, nxt=nxt, o=o, g=g: V.tensor_tensor(
                out=nxt[:, 0:TOK], in0=cur[:, o:o + TOK], in1=invcnt[:, g, :], op=ALU.mult),
                r=[Bcur, B_inv], w=[Bn])
            fw.op(dve, lambda nxt=nxt, cc=cc: V.tensor_tensor(
                out=pooledT[:, cc, :], in0=nxt[:, 0:TOK], in1=upool[:, 8:8 + TOK], op=ALU.subtract),
                r=[Bn, B_upool], w=[B_pooled[cc]])
            if cc == 7:
                for gg in range(4):
                    for dc in range(2):
                        for half in range(2):
                            bk = nextbank()
                            for kc in range(2):
                                fw.op(pe, lambda gg=gg, dc=dc, half=half, kc=kc, bk=bk: T.matmul(
                                    PS[bk][:, :], poolw[:, gg * 2 + kc, dc * 128:(dc + 1) * 128],
                                    pooledT[:, gg * 2 + kc, half * 512:(half + 1) * 512],
                                    start=(kc == 0), stop=(kc == 1)),
                                    r=[B_poolw, B_pooled[gg * 2 + kc]],
                                    w=[BPS[bk]] if kc == 0 else (), pw=[BPS[bk]] if kc else (), inc=(kc == 1))
                            ch = gg * 2 + dc
                            fw.op(dve, lambda ch=ch, half=half, bk=bk: V.tensor_scalar(
                                out=mixedT[:, ch, half * 512:(half + 1) * 512], in0=PS[bk][:, :],
                                scalar1=pscale_s[:, ch:ch + 1], scalar2=None, op0=ALU.mult),
                                r=[BPS[bk], B_pscale], pw=[B_mixed[ch]])
        elif cc < 24:
            isq = cc < 16
            h = cc - 8 if isq else cc - 16
            for half in range(2):
                bk = nextbank()
                proj_T(cc, 8 + half * 512, 512, bk)
                ra, rb = ropea[half], ropeb[half]
                fw.op(dve, lambda bk=bk, ra=ra, half=half: V.tensor_tensor(
                    out=ra[:], in0=PS[bk][:, :], in1=cosT[:, half * 512:(half + 1) * 512], op=ALU.mult),
                    r=[BPS[bk], B_cs], w=[B_ropea[half]])
                fw.op(dve, lambda bk=bk, rb=rb, half=half: V.tensor_tensor(
                    out=rb[:], in0=PS[bk][:, :], in1=sinT[:, half * 512:(half + 1) * 512], op=ALU.mult),
                    r=[BPS[bk], B_cs], w=[B_ropeb[half]])
                b2 = nextbank(4, 8)
                fw.op(pe, lambda b2=b2, ra=ra: T.matmul(PS[b2][:, :], ident_b[:], ra[:], start=True, stop=False),
                      r=[B_identb, B_ropea[half]], w=[BPS[b2]], inc=False)
                fw.op(pe, lambda b2=b2, rb=rb: T.matmul(PS[b2][:, :], Rm_b[:], rb[:], start=False, stop=True),
                      r=[B_Rm, B_ropeb[half]], pw=[BPS[b2]])
                if isq:
                    fw.op(act, lambda b2=b2, h=h, half=half: A.mul(
                        out=qT[:, h, half * 512:(half + 1) * 512], in_=PS[b2][:, :], mul=0.125),
                        r=[BPS[b2]], pw=[B_q[h]])
                else:
                    ks, Bk = kst[h % 2], B_kst[h % 2]
                    fw.op(act, lambda b2=b2, ks=ks, half=half: A.copy(
                        out=ks[:, half * 512:(half + 1) * 512], in_=PS[b2][:, :]),
                        r=[BPS[b2]], pw=[Bk])
            if not isq:
                fw.dma(sp, kvloc[h * 128:(h + 1) * 128, :], kst[h % 2][:], B_kst[h % 2],
                       r=[B_kst[h % 2]], pw=[B_kvloc])
        else:
            h = cc - 24
            s = slot_of[cc]
            vs, Bv = vst[h % 2], B_vst[h % 2]
            for tg in range(2):
                bk = nextbank()
                for t4 in range(4):
                    tt = tg * 4 + t4
                    for k in range(16):
                        fw.op(pe, lambda k=k, tt=tt, t4=t4, bk=bk, s=s: T.matmul(
                            PS[bk][:, t4 * 128:(t4 + 1) * 128], xT_s[:, k, 8 + tt * 128:8 + (tt + 1) * 128],
                            wring[s][:, k, :], start=(k == 0), stop=(k == 15)),
                            r=[B_wr[s], B_xT],
                            w=[BPS[bk]] if (k == 0 and t4 == 0) else (),
                            pw=[BPS[bk]] if not (k == 0 and t4 == 0) else (),
                            inc=(k == 15 and t4 == 3))
                fw.op(act, lambda bk=bk, vs=vs, tg=tg: A.copy(
                    out=vs[:, tg * 4:(tg + 1) * 4, :].rearrange("p a b -> p (a b)"), in_=PS[bk][:, :]),
                    r=[BPS[bk]], pw=[Bv])
            fw.dma(sp, kvloc[1024 + h * 128:1024 + (h + 1) * 128, :], vs[:].rearrange("p a b -> p (a b)"),
                   Bv, r=[Bv], pw=[B_kvloc])
            if cc == 31 and stage != 0.05:
                fw.coll(kvloc[:, :], kvall[:, :], r=[B_kvloc], w=[B_kvall])

    if stage == 0.05:
        return early_exit([s1, sA])
    if stage == 0.1:
        return early_exit([s1, sA])
    fw.barrier()
    s1.close()

    s2 = contextlib.ExitStack()
    kTh = [sbt(s2, f"kTh{i}", [128, S], BF16) for i in range(2)]; B_kTh = [Buf("kTh0"), Buf("kTh1")]
    vh = [sbt(s2, f"vh{i}", [128, 32, 128], BF16) for i in range(2)]; B_vh = [Buf("vh0"), Buf("vh1")]
    pT = [sbt(s2, f"pT{i}", [128, 512], BF16) for i in range(3)]; B_pT = [Buf(f"pT{i}") for i in range(3)]
    ikv = [sbt(s2, f"ikv{c}", [128, 1], I32) for c in range(64)]; B_idxKV = Buf("idxKV")
    lam_s = sbt(s2, "lam_s", [128, 256], F32); B_lam = Buf("lam")
    lamv = sbt(s2, "lamv", [128, 8], F32); B_lamv = Buf("lamv")
    subg_s = sbt(s2, "subg_s", [128, 128], F32); B_subg = Buf("subg")
    ot = [sbt(s2, f"ot{i}", [128, 128], F32) for i in range(4)]; B_ot = [Buf(f"ot{i}") for i in range(4)]
    osm = sbt(s2, "osm", [128, 16], F32); B_osm = Buf("osm")
    ao = sbt(s2, "ao", [128, 128], BF16); B_ao = Buf("ao")
    junk = sbt(s2, "junk", [128, 128], F32); B_junk = Buf("junk")

    for c in range(64):
        fw.dma(sp, ikv[c][:], idxKV_d[c], B_idxKV, pw=[B_idxKV])
    fw.dma(sp, lam_s[:], lam_in[:, :], B_lam, w=[B_lam])
    fw.dma(sp, subg_s[:], subg[:, :], B_subg, w=[B_subg])
    for i in range(2):
        fw.op(dve, lambda i=i: V.tensor_tensor(out=junk[:, 0:64], in0=lam_s[:, i * 128:i * 128 + 64],
                                               in1=lam_s[:, i * 128 + 64:i * 128 + 128], op=ALU.mult),
              r=[B_lam], w=[B_junk])
        fw.op(dve, lambda i=i: V.tensor_reduce(out=lamv[:, i:i + 1], in_=junk[:, 0:64], axis=AX.X, op=ALU.add),
              r=[B_junk], pw=[B_lamv])
    fw.op(act, lambda: A.activation(out=lamv[:, 2:4], in_=lamv[:, 0:2], func=AF.Exp), r=[B_lamv], pw=[B_lamv])
    fw.op(dve, lambda: V.tensor_tensor(out=lamv[:, 4:5], in0=lamv[:, 3:4], in1=lamv[:, 2:3], op=ALU.subtract),
          r=[B_lamv], pw=[B_lamv])
    fw.op(dve, lambda: V.tensor_scalar(out=lamv[:, 5:6], in0=lamv[:, 4:5], scalar1=-LAM_INIT, scalar2=None,
                                       op0=ALU.add), r=[B_lamv], pw=[B_lamv])

    icol = [sbt(s2, f"icol{i}", [128, 1], I32) for i in range(4)]; B_icol = [Buf(f"icol{i}") for i in range(4)]
    vtmp = [sbt(s2, f"vtmp{i}", [128, 1024], BF16) for i in range(2)]; B_vtmp = [Buf("vtmp0"), Buf("vtmp1")]
    ic_i = [0]

    def load_head(h):
        s = h % 2
        for r4 in range(4):
            c = h * 4 + r4
            fw.dma(pool, kTh[s][:, r4 * 1024:(r4 + 1) * 1024], kvall[:, :], B_kTh[s],
                   r=[B_kvall, B_idxKV], pw=[B_kTh[s]], indirect=ikv[c][:, :])
            fw.dma(pool, vh[s][:, r4 * 8:(r4 + 1) * 8, :].rearrange("p a b -> p (a b)"), kvall[:, :], B_vh[s],
                   r=[B_kvall, B_idxKV], pw=[B_vh[s]], indirect=ikv[32 + c][:, :])

    load_head(0)
    if stage == 0.15:
        return early_exit([s2, sA])
    SB = (0, 1, 2)
    ones_b = sbt(s2, "ones_b", [128, 128], BF16); B_onesb = Buf("onesb")
    ones_f = sbt(s2, "ones_f", [128, 128], F32); B_onesf = Buf("onesf")
    fw.op(dve, lambda: V.memset(ones_b[:], 1.0), w=[B_onesb])
    fw.op(dve, lambda: V.memset(ones_f[:], 1.0), w=[B_onesf])
    subgc = sbt(s2, "subgc_s", [128, 1], F32); B_subgc = Buf("subgc")
    fw.dma(sp, subgc[:], subgc_d[:, :], B_subgc, w=[B_subgc])
    fin = [sbt(s2, f"fin{i}", [128, 512], F32) for i in range(6)]; B_fin = [Buf(f"fin{i}") for i in range(6)]
    sqh = sbt(s2, "sqh", [128, 512], BF16); B_sqh = Buf("sqh")
    sql = sbt(s2, "sql", [128, 512], BF16); B_sql = Buf("sql")
    steps = [(h, qh, c, kt) for h in range(8) for qh in range(2) for c in range(2) for kt in range(32)]
    LAG = 2

    def emit_qk(i):
        h, qh, c, kt = steps[i]
        s = h % 2
        sb_ = SB[i % 3]
        pt, Bp = pT[i % 3], B_pT[i % 3]
        fw.op(pe, lambda: T.matmul(
            PS[sb_][:, :], kTh[s][c * 64:(c + 1) * 64, kt * 128:(kt + 1) * 128],
            qT[c * 64:(c + 1) * 64, h, qh * 512:(qh + 1) * 512], start=True, stop=True),
            r=[B_kTh[s], B_q[h]], w=[BPS[sb_]])
        fw.op(act, lambda: A.activation(out=pt[:], in_=PS[sb_][:, :], func=AF.Exp),
              r=[BPS[sb_]], w=[Bp])

    def emit_pv(i):
        h, qh, c, kt = steps[i]
        s = h % 2
        pt, Bp = pT[i % 3], B_pT[i % 3]
        fw.op(pe, lambda: T.matmul(
            PS[3 + c][:, :], vh[s][:, kt, :], pt[:], start=(kt == 0), stop=(kt == 31)),
            r=[Bp, B_vh[s]], w=[BPS[3 + c]] if kt == 0 else (), pw=[BPS[3 + c]] if kt else (),
            inc=False)
        fw.op(pe, lambda: T.matmul(
            PS[5 + c][:, :], ones_b[:], pt[:], start=(kt == 0), stop=(kt == 31)),
            r=[Bp, B_onesb], w=[BPS[5 + c]] if kt == 0 else (), pw=[BPS[5 + c]] if kt else ())

    def finalize(h, qh):

        rc0, rc1, o0, t_, o_, sq = fin
        Brc0, Brc1, Bo0, Bt_, Bo_, Bsq = B_fin
        fw.op(dve, lambda: V.reciprocal(out=rc0[:], in_=PS[5][:, :]), r=[BPS[5]], w=[Brc0])
        fw.op(dve, lambda: V.reciprocal(out=rc1[:], in_=PS[6][:, :]), r=[BPS[6]], w=[Brc1])
        fw.op(dve, lambda: V.tensor_tensor(out=o0[:], in0=PS[3][:, :], in1=rc0[:], op=ALU.mult),
              r=[BPS[3], Brc0], w=[Bo0])
        fw.op(dve, lambda: V.tensor_tensor(out=t_[:], in0=PS[4][:, :], in1=rc1[:], op=ALU.mult),
              r=[BPS[4], Brc1], w=[Bt_])
        fw.op(dve, lambda: V.scalar_tensor_tensor(out=o_[:], in0=t_[:], scalar=lamv[:, 5:6], in1=o0[:],
                                                  op0=ALU.mult, op1=ALU.add),
              r=[Bt_, Bo0, B_lamv], w=[Bo_])
        fw.op(dve, lambda: V.tensor_tensor(out=sq[:], in0=o_[:], in1=o_[:], op=ALU.mult), r=[Bo_], w=[Bsq])
        fw.op(dve, lambda: V.tensor_copy(out=sqh[:], in_=sq[:]), r=[Bsq], w=[B_sqh])
        fw.op(dve, lambda: V.tensor_tensor(out=sql[:], in0=sq[:], in1=sqh[:], op=ALU.subtract),
              r=[Bsq, B_sqh], w=[B_sql])
        fw.op(pe, lambda: T.matmul(PS[7][:, :], ones_b[:], sqh[:], start=True, stop=False),
              r=[B_onesb, B_sqh], w=[BPS[7]], inc=False)
        fw.op(pe, lambda: T.matmul(PS[7][:, :], ones_b[:], sql[:], start=False, stop=True),
              r=[B_onesb, B_sql], pw=[BPS[7]])
        fw.op(dve, lambda: V.tensor_scalar(out=rc0[:], in0=PS[7][:, :], scalar1=1.0 / 128.0, scalar2=1e-5,
                                           op0=ALU.mult, op1=ALU.add), r=[BPS[7]], w=[Brc0])
        fw.op(act, lambda: A.sqrt(out=rc1[:], in_=rc0[:]), r=[Brc0], w=[Brc1])
        fw.op(dve, lambda: V.reciprocal(out=rc0[:], in_=rc1[:]), r=[Brc1], w=[Brc0])
        fw.op(dve, lambda: V.tensor_tensor(out=t_[:], in0=o_[:], in1=rc0[:], op=ALU.mult),
              r=[Bo_, Brc0], w=[Bt_])
        fw.op(dve, lambda h=h, qh=qh: V.tensor_scalar(
            out=mixedT[:, 8 + h, qh * 512:(qh + 1) * 512], in0=t_[:], scalar1=subgc[:, 0:1],
            scalar2=(1.0 - LAM_INIT), op0=ALU.mult, op1=ALU.mult),
            r=[Bt_, B_subgc], pw=[B_mixed[8 + h]])


    NS = len(steps)
    for i in range(NS + LAG):
        j = i - LAG
        if j >= 0:
            h, qh, c, kt = steps[j]
            if qh == 0 and c == 0 and kt == 0 and h + 1 < 8:
                load_head(h + 1)
            emit_pv(j)
        if i < NS:
            emit_qk(i)
        if j >= 0 and c == 1 and kt == 31:
            finalize(h, qh)

    if stage == 0.2:
        return early_exit([s2, sA])
    fw.barrier()
    s2.close()

    s3 = contextlib.ExitStack()
    wout = sbt(s3, "wout_s", [128, 16, D], BF16); B_wout = Buf("wout")
    g1 = sbt(s3, "g1", [128, D], F32); b1 = sbt(s3, "b1", [128, D], F32); B_ln1 = Buf("ln1")
    wr_s = sbt(s3, "wr_s", [128, 16, 16], F32); B_wrs = Buf("wr")
    xt_ = [sbt(s3, f"xt{i}", [128, D], F32) for i in range(2)]; B_xt = [Buf("xt0"), Buf("xt1")]
    rt = [sbt(s3, f"rt{i}", [128, D], F32) for i in range(2)]; B_rt = [Buf("rt0"), Buf("rt1")]
    x1b = [sbt(s3, f"x1b{i}", [128, D], BF16) for i in range(2)]; B_x1b = [Buf("x1b0"), Buf("x1b1")]
    x1T = sbt(s3, "x1T", [128, 16, 128], F32); B_x1T = Buf("x1T")
    stats = sbt(s3, "stats", [128, 4, 6], F32); B_stats = Buf("stats")
    mv = sbt(s3, "mv", [128, 8], F32); B_mv = Buf("mv")
    aff_s = sbt(s3, "aff_s", [128, 8, 16], F32); B_affs = Buf("affs")
    lsm = sbt(s3, "lsm", [128, 4], F32); B_lsm = Buf("lsm")

    for k in range(16):
        fw.dma(pool, wout[:, k, :], w_out_r[:, k * D:(k + 1) * D], B_wout, pw=[B_wout])
    fw.dma(sp, g1[:], ln1g[:, :], B_ln1, pw=[B_ln1])
    fw.dma(sp, b1[:], ln1b[:, :], B_ln1, pw=[B_ln1])
    fw.dma(sp, wr_s[:].rearrange("p a b -> p (a b)"), wr_r[:, :], B_wrs, w=[B_wrs])

    def layer_norm(r_t, B_r, gam, bet, B_gb, out_t, B_out):
        for c4 in range(4):
            fw.op(dve, lambda c4=c4: V.bn_stats(out=stats[:, c4, :], in_=r_t[:, c4 * 512:(c4 + 1) * 512]),
                  r=[B_r], pw=[B_stats])
        fw.op(dve, lambda: V.bn_aggr(out=mv[:, 0:2], in_=stats[:].rearrange("p a b -> p (a b)")),
              r=[B_stats], pw=[B_mv])
        fw.op(dve, lambda: V.tensor_scalar(out=mv[:, 2:3], in0=mv[:, 1:2], scalar1=LN_EPS, scalar2=None,
                                           op0=ALU.add), r=[B_mv], pw=[B_mv])
        fw.op(act, lambda: A.sqrt(out=mv[:, 3:4], in_=mv[:, 2:3]), r=[B_mv], pw=[B_mv])
        fw.op(dve, lambda: V.reciprocal(out=mv[:, 4:5], in_=mv[:, 3:4]), r=[B_mv], pw=[B_mv])
        fw.op(dve, lambda: V.tensor_scalar(out=r_t[:], in0=r_t[:], scalar1=mv[:, 0:1], scalar2=mv[:, 4:5],
                                           op0=ALU.subtract, op1=ALU.mult), r=[B_mv], w=[B_r])
        fw.op(dve, lambda: V.tensor_tensor(out=r_t[:], in0=r_t[:], in1=gam[:], op=ALU.mult),
              r=[B_gb], w=[B_r])
        fw.op(dve, lambda: V.tensor_tensor(out=out_t[:], in0=r_t[:], in1=bet[:], op=ALU.add),
              r=[B_gb, B_r], w=[B_out])

    for tt in range(8):
        xs, Bx = xt_[tt % 2], B_xt[tt % 2]
        rr, Br = rt[tt % 2], B_rt[tt % 2]
        fw.dma(sp, xs[:], xtok[tt * 128:(tt + 1) * 128, :], Bx, w=[Bx])
        for dc in range(4):
            for k in range(16):
                fw.op(pe, lambda k=k, dc=dc, tt=tt: T.matmul(
                    PS[dc][:, :], mixedT[:, k, tt * 128:(tt + 1) * 128], wout[:, k, dc * 512:(dc + 1) * 512],
                    start=(k == 0), stop=(k == 15)),
                    r=[B_mixed[k], B_wout], w=[BPS[dc]] if k == 0 else (), pw=[BPS[dc]] if k else (),
                    inc=(k == 15))
            fw.op(dve, lambda dc=dc, xs=xs, rr=rr: V.scalar_tensor_tensor(
                out=rr[:, dc * 512:(dc + 1) * 512], in0=xs[:, dc * 512:(dc + 1) * 512], scalar=ALPHA,
                in1=PS[dc][:, :], op0=ALU.mult, op1=ALU.add),
                r=[Bx, BPS[dc]], pw=[Br])
        layer_norm(rr, Br, g1, b1, B_ln1, xs, Bx)
        fw.dma(sp, x1f[tt * 128:(tt + 1) * 128, :], xs[:], Bx, r=[Bx], pw=[B_x1f])
        xb, Bxb = x1b[tt % 2], B_x1b[tt % 2]
        fw.op(act, lambda xb=xb, xs=xs: A.copy(out=xb[:], in_=xs[:]), r=[Bx], w=[Bxb])
        fw.dma(sp, x1bf[tt * 128:(tt + 1) * 128, :], xb[:], Bxb, r=[Bxb], pw=[B_x1bf])
        for k4 in range(4):
            tb = 4 + (k4 % 2)
            for j in range(4):
                k = k4 * 4 + j
                fw.op(pe, lambda tb=tb, j=j, k=k, xs=xs: T.transpose(
                    PS[tb][:, j * 128:(j + 1) * 128], xs[:, k * 128:(k + 1) * 128], ident_f[:]),
                    r=[Bx, B_identf], w=[BPS[tb]] if j == 0 else (), pw=[BPS[tb]] if j else (), inc=(j == 3))
            fw.op(act, lambda tb=tb, k4=k4: A.copy(
                out=x1T[:, k4 * 4:(k4 + 1) * 4, :].rearrange("p a b -> p (a b)"), in_=PS[tb][:, :]),
                r=[BPS[tb]], pw=[B_x1T])
        for k in range(16):
            fw.op(pe, lambda k=k: T.matmul(PS[6][:, 0:16], x1T[:, k, :], wr_s[:, k, :],
                                           start=(k == 0), stop=(k == 15)),
                  r=[B_x1T, B_wrs], w=[BPS[6]] if k == 0 else (), pw=[BPS[6]] if k else (), inc=(k == 15))
        fw.op(dve, lambda: V.tensor_reduce(out=lsm[:, 0:1], in_=PS[6][:, 0:16], axis=AX.X, op=ALU.max),
              r=[BPS[6]], pw=[B_lsm])
        fw.op(dve, lambda: V.tensor_scalar(out=lsm[:, 1:2], in0=lsm[:, 0:1], scalar1=-1.0, scalar2=None,
                                           op0=ALU.mult), r=[B_lsm], pw=[B_lsm])
        fw.op(act, lambda tt=tt: A.activation(out=aff_s[:, tt, :], in_=PS[6][:, 0:16], func=AF.Exp,
                                              bias=lsm[:, 1:2], accum_out=lsm[:, 2:3]),
              r=[BPS[6], B_lsm], pw=[B_affs, B_lsm])
        fw.op(dve, lambda: V.reciprocal(out=lsm[:, 3:4], in_=lsm[:, 2:3]), r=[B_lsm], pw=[B_lsm])
        fw.op(dve, lambda tt=tt: V.tensor_scalar(out=aff_s[:, tt, :], in0=aff_s[:, tt, :], scalar1=lsm[:, 3:4],
                                                 scalar2=None, op0=ALU.mult), r=[B_lsm], w=[B_affs])
    fw.dma(sp, affloc[:, :].rearrange("(a p) e -> p a e", p=128), aff_s[:], B_affs, r=[B_affs], pw=[B_affloc])
    fw.coll(x1bf[:, :], x1all[:, :], r=[B_x1bf], w=[B_x1all])
    fw.coll(affloc[:, :], affall[:, :], r=[B_affloc], w=[B_affall])
    fw.barrier()
    s3.close()
    sA.close()

    if stage <= 1:
        sd = contextlib.ExitStack()
        t_ = sbt(sd, "dbgt", [128, D], F32); Bt = Buf("dbgt")
        for tt in range(8):
            fw.dma(sp, t_[:], x1f[tt * 128:(tt + 1) * 128, :], Bt, r=[B_x1f], w=[Bt])
            fw.dma(sp, out_d[tt * 128:(tt + 1) * 128, :], t_[:], Bt, r=[Bt])
        fw.barrier()
        sd.close()
        top.close()
        return nc

    sB = contextlib.ExitStack()
    idxs = sbt(sB, "idxs", [128, 16], I32); B_idxs = Buf("idxs")
    gate = sbt(sB, "gate", [128, 16], F32); B_gate = Buf("gate")

    sb1 = contextlib.ExitStack()
    Ab = [sbt(sb1, f"Ab{b}", [128, 32, 16], F32) for b in range(2)]; B_Ab = [Buf("Ab0"), Buf("Ab1")]
    sel_s = sbt(sb1, "sel_s", [128, 2, 16], F32); B_sel = Buf("sel")
    tmpA = sbt(sb1, "tmpA", [128, 32, 16], F32); B_tmpA = Buf("tmpA")
    vg = sbt(sb1, "vg", [128, 4, 32], F32); B_vg = Buf("vg")
    lo = sbt(sb1, "lo", [128, 4], F32); hi = sbt(sb1, "hi", [128, 4], F32); mid = sbt(sb1, "mid", [128, 4], F32)
    cntp = sbt(sb1, "cntp", [128, 4], F32); ge = sbt(sb1, "ge", [128, 4], F32)
    t1 = sbt(sb1, "t1", [128, 4], F32)
    B_lo, B_hi, B_mid, B_cntp, B_ge, B_t1 = Buf("lo"), Buf("hi"), Buf("mid"), Buf("cntp"), Buf("ge"), Buf("t1")
    jk = sbt(sb1, "jk", [128, 32], F32); B_jk = Buf("jk")
    ones_s = sbt(sb1, "ones_s", [128, 128], F32); ltri_s = sbt(sb1, "ltri_s", [128, 128], F32)
    B_ones, B_ltri = Buf("ones"), Buf("ltri")
    iota_s = sbt(sb1, "iota_s", [128, 512], F32); B_iota = Buf("iota")
    tdec = sbt(sb1, "tdec_s", [128, 2, 32, 2], F32); B_tdec = Buf("tdec")
    coreoff = sbt(sb1, "coreoff_s", [128, 1], F32); B_coreoff = Buf("coreoff")
    maskg = sbt(sb1, "maskg", [128, 4, 32], F32); B_mask = Buf("mask")
    incl = sbt(sb1, "incl", [128, 4, 32], F32); B_incl = Buf("incl")
    posg = sbt(sb1, "posg", [128, 4, 32], F32); B_pos = Buf("pos")
    onesr = sbt(sb1, "onesr", [128, 32], F32); B_onesr = Buf("onesr")
    Rg = sbt(sb1, "Rg", [128, 4, 32, 4], F32); B_Rg = Buf("Rg")
    PMs = [sbt(sb1, f"PMs{b}", [128, 32, 4], F32) for b in range(2)]; B_PMs = [Buf("PMs0"), Buf("PMs1")]
    OH = [sbt(sb1, f"OH{i}", [128, 512], F32) for i in range(2)]; B_OH = [Buf("OH0"), Buf("OH1")]
    slf = sbt(sb1, "slf", [128, 4], F32); B_slf = Buf("slf")

    for b in range(2):
        fw.dma(sp, Ab[b][:].rearrange("p a e -> p (a e)"),
               affall[b * S:(b + 1) * S, :].rearrange("(p a) e -> p (a e)", p=128), B_Ab[b],
               r=[B_affall], w=[B_Ab[b]])
    fw.dma(sp, sel_s[:].rearrange("p a e -> p (a e)"), sel_d[:, :], B_sel, w=[B_sel])
    fw.dma(sp, ones_s[:], ones_d[:, :], B_ones, w=[B_ones])
    fw.dma(sp, ltri_s[:], ltri_d[:, :], B_ltri, w=[B_ltri])
    fw.dma(sp, iota_s[:], iota_d[:, :], B_iota, w=[B_iota])
    fw.dma(sp, tdec[:].rearrange("p a b c -> p (a b c)"), tdec_d[:, :], B_tdec, w=[B_tdec])
    fw.dma(sp, coreoff[:], coreoff_d[:, :], B_coreoff, w=[B_coreoff])
    fw.op(dve, lambda: V.memset(onesr[:], 1.0), w=[B_onesr])
    fw.op(dve, lambda: V.memset(Rg[:].rearrange("p a b c -> p (a b c)"), 0.0), w=[B_Rg])
    for g in range(4):
        el, b = g // 2, g % 2
        fw.op(dve, lambda el=el, b=b: V.tensor_tensor(
            out=tmpA[:], in0=Ab[b][:], in1=sel_s[:, el:el + 1, :].to_broadcast([128, 32, 16]), op=ALU.mult),
            r=[B_Ab[b], B_sel], w=[B_tmpA])
        fw.op(dve, lambda g=g: V.tensor_reduce(out=vg[:, g, :], in_=tmpA[:], axis=AX.X, op=ALU.add),
              r=[B_tmpA], pw=[B_vg])
    fw.op(dve, lambda: V.memset(lo[:], 0.0), w=[B_lo])
    fw.op(dve, lambda: V.memset(hi[:], 1.0), w=[B_hi])
    for it in range(NBIS):
        fw.op(dve, lambda: V.tensor_tensor(out=mid[:], in0=lo[:], in1=hi[:], op=ALU.add), r=[B_lo, B_hi], w=[B_mid])
        fw.op(dve, lambda: V.tensor_scalar(out=mid[:], in0=mid[:], scalar1=0.5, scalar2=None, op0=ALU.mult),
              w=[B_mid])
        for g in range(4):
            fw.op(dve, lambda g=g: V.tensor_scalar(out=jk[:], in0=vg[:, g, :], scalar1=mid[:, g:g + 1], scalar2=None,
                                                   op0=ALU.is_ge, op1=ALU.add, accum_out=cntp[:, g:g + 1]),
                  r=[B_vg, B_mid], w=[B_jk], pw=[B_cntp])
        fw.op(pe, lambda: T.matmul(PS[0][:, 0:4], ones_s[:], cntp[:], start=True, stop=True),
              r=[B_ones, B_cntp], w=[BPS[0]])
        fw.op(dve, lambda: V.tensor_scalar(out=ge[:], in0=PS[0][:, 0:4], scalar1=CAP - 0.5, scalar2=None,
                                           op0=ALU.is_ge), r=[BPS[0]], w=[B_ge])
        fw.op(dve, lambda: V.tensor_tensor(out=t1[:], in0=ge[:], in1=mid[:], op=ALU.mult), r=[B_ge, B_mid], w=[B_t1])
        fw.op(dve, lambda: V.tensor_tensor(out=lo[:], in0=lo[:], in1=t1[:], op=ALU.max), r=[B_t1], w=[B_lo])
        fw.op(dve, lambda: V.scalar_tensor_tensor(out=t1[:], in0=ge[:], scalar=2.0, in1=mid[:], op0=ALU.mult,
                                                  op1=ALU.add), r=[B_ge, B_mid], w=[B_t1])
        fw.op(dve, lambda: V.tensor_tensor(out=hi[:], in0=hi[:], in1=t1[:], op=ALU.min), r=[B_t1], w=[B_hi])
    for g in range(4):
        fw.op(dve, lambda g=g: V.tensor_scalar(out=maskg[:, g, :], in0=vg[:, g, :], scalar1=lo[:, g:g + 1],
                                               scalar2=None, op0=ALU.is_ge), r=[B_vg, B_lo], pw=[B_mask])
    for g in range(4):
        fw.op(dve, lambda g=g: V.tensor_tensor_scan(out=incl[:, g, :], data0=onesr[:], data1=maskg[:, g, :],
                                                    initial=0.0, op0=ALU.mult, op1=ALU.add),
              r=[B_onesr, B_mask], pw=[B_incl])
    fw.op(dve, lambda: V.tensor_copy(out=cntp[:], in_=incl[:, :, 31]), r=[B_incl], w=[B_cntp])
    fw.op(pe, lambda: T.matmul(PS[1][:, 0:4], ltri_s[:], cntp[:], start=True, stop=True),
          r=[B_ltri, B_cntp], w=[BPS[1]])
    fw.op(dve, lambda: V.tensor_copy(out=slf[:], in_=PS[1][:, 0:4]), r=[BPS[1]], w=[B_slf])
    for g in range(4):
        el, b = g // 2, g % 2
        fw.op(dve, lambda g=g: V.tensor_scalar(out=posg[:, g, :], in0=incl[:, g, :], scalar1=slf[:, g:g + 1],
                                               scalar2=None, op0=ALU.add), r=[B_incl, B_slf], pw=[B_pos])
        fw.op(dve, lambda g=g: V.tensor_tensor(out=posg[:, g, :], in0=posg[:, g, :], in1=maskg[:, g, :],
                                               op=ALU.subtract), r=[B_mask], w=[B_pos])
        fw.op(dve, lambda g=g, el=el, b=b: V.tensor_scalar(out=PMs[b][:, :, el], in0=posg[:, g, :],
                                                           scalar1=coreoff[:, 0:1], scalar2=float(g * 512),
                                                           op0=ALU.add, op1=ALU.add),
              r=[B_pos, B_coreoff], pw=[B_PMs[b]])
        fw.op(dve, lambda el=el, b=b: V.tensor_scalar(out=PMs[b][:, :, el], in0=PMs[b][:, :, el],
                                                      scalar1=float(NCORES * 2048 - 1), scalar2=None, op0=ALU.min),
              w=[B_PMs[b]])
        fw.op(dve, lambda g=g, el=el, b=b: V.tensor_copy(out=PMs[b][:, :, 2 + el], in_=maskg[:, g, :]),
              r=[B_mask], w=[B_PMs[b]])
        fw.op(dve, lambda g=g, b=b: V.tensor_copy(out=Rg[:, g, :, 0:2], in_=tdec[:, b, :, :]),
              r=[B_tdec], pw=[B_Rg])
        fw.op(dve, lambda g=g: V.tensor_copy(out=Rg[:, g, :, 2], in_=vg[:, g, :]), r=[B_vg], pw=[B_Rg])
    for b in range(2):
        fw.dma(sp, pmloc[b * S:(b + 1) * S, :].rearrange("(p a) e -> p (a e)", p=128),
               PMs[b][:].rearrange("p a e -> p (a e)"), B_PMs[b], r=[B_PMs[b]], pw=[B_pmloc])
    fw.coll(pmloc[:, :], pmall[:, :], r=[B_pmloc], w=[B_pmall])
    for g in range(4):
        for j in range(32):
            oh, Bo = OH[j % 2], B_OH[j % 2]
            fw.op(dve, lambda g=g, j=j, oh=oh: V.tensor_scalar(
                out=oh[:], in0=iota_s[:], scalar1=posg[:, g, j:j + 1], scalar2=maskg[:, g, j:j + 1],
                op0=ALU.is_equal, op1=ALU.mult), r=[B_iota, B_pos, B_mask], w=[Bo])
            for sc in range(4):
                fw.op(pe, lambda g=g, j=j, sc=sc, oh=oh: T.matmul(
                    PS[4 + sc][:, 0:4], oh[:, sc * 128:(sc + 1) * 128], Rg[:, g, j, :],
                    start=(j == 0), stop=(j == 31)),
                    r=[Bo, B_Rg], w=[BPS[4 + sc]] if j == 0 else (), pw=[BPS[4 + sc]] if j else (),
                    inc=(sc == 3))
        for sc in range(4):
            col = g * 4 + sc
            fw.op(dve, lambda sc=sc: V.tensor_copy(out=t1[:], in_=PS[4 + sc][:, 0:4]), r=[BPS[4 + sc]], w=[B_t1])
            fw.op(dve, lambda sc=sc: V.scalar_tensor_tensor(out=slf[:, 0:1], in0=t1[:, 0:1], scalar=64.0,
                                                            in1=t1[:, 1:2], op0=ALU.mult, op1=ALU.add),
                  r=[B_t1], w=[B_slf])
            fw.op(dve, lambda col=col: V.tensor_copy(out=idxs[:, col:col + 1], in_=slf[:, 0:1]),
                  r=[B_slf], pw=[B_idxs])
            fw.op(dve, lambda col=col, sc=sc: V.tensor_copy(out=gate[:, col:col + 1], in_=t1[:, 2:3]),
                  r=[B_t1], pw=[B_gate])
    fw.barrier()
    sb1.close()

    xinT = sbt(sB, "xinT", [128, 16, 1024], BF16); B_xin = Buf("xin")
    hT = sbt(sB, "hT", [128, NF, 1024], BF16); B_hT = [Buf(f"hT{f}") for f in range(NF)]
    xg = [sbt(sB, f"xg{i}", [128, D], BF16) for i in range(2)]; B_xg = [Buf("xg0"), Buf("xg1")]
    NGU = 3
    wgu = [sbt(sB, f"wgu{i}", [128, 2, 16, 128], BF16) for i in range(NGU)]; B_wgu = [Buf(f"wgu{i}") for i in range(NGU)]
    NWD = 6
    wdn = [sbt(sB, f"wdn{i}", [128, 512], BF16) for i in range(NWD)]; B_wdn = [Buf(f"wdn{i}") for i in range(NWD)]
    sg = [sbt(sB, f"sg{i}", [128, 512], F32) for i in range(2)]; B_sg = [Buf("sg0"), Buf("sg1")]
    yst = [sbt(sB, f"yst{i}", [128, 512], F32) for i in range(4)]; B_yst = [Buf(f"yst{i}") for i in range(4)]

    icolB = [sbt(sB, f"icolB{i}", [128, 1], I32) for i in range(4)]; B_icolB = [Buf(f"icolB{i}") for i in range(4)]
    icb_i = [0]
    gu_i = [0]
    wd_i = [0]
    yst_i = [0]
    for el in range(2):
        for b in range(2):
            g = el * 2 + b
            for sc in range(4):
                col = g * 4 + sc
                xq, Bq = xg[sc % 2], B_xg[sc % 2]
                ii = icb_i[0] % 4; icb_i[0] += 1
                fw.op(dve, lambda ii=ii, col=col: V.tensor_copy(out=icolB[ii][:], in_=idxs[:, col:col + 1]),
                      r=[B_idxs], w=[B_icolB[ii]])
                harden(icolB[ii], B_icolB[ii])
                fw.dma(pool, xq[:], x1all[:, :], Bq, r=[B_x1all, B_icolB[ii]], w=[Bq], indirect=icolB[ii][:, :])
                s0 = b * 512 + sc * 128
                for k4 in range(4):
                    tb = 6 + (k4 % 2)
                    for j in range(4):
                        k = k4 * 4 + j
                        fw.op(pe, lambda tb=tb, j=j, k=k, xq=xq: T.transpose(
                            PS[tb][:].bitcast(BF16)[:, j * 128:(j + 1) * 128], xq[:, k * 128:(k + 1) * 128],
                            ident_b[:]),
                            r=[Bq, B_identb], w=[BPS[tb]] if j == 0 else (), pw=[BPS[tb]] if j else (),
                            inc=(j == 3))
                    fw.op(act, lambda tb=tb, k4=k4, s0=s0: A.copy(
                        out=xinT[:, k4 * 4:(k4 + 1) * 4, s0:s0 + 128],
                        in_=PS[tb][:].bitcast(BF16)[:, 0:512].rearrange("p (a b) -> p a b", a=4)),
                        r=[BPS[tb]], pw=[B_xin])
        def load_gu(f):
            i = gu_i[0]; gu_i[0] += 1
            s = i % NGU
            fw.dma(pool, wgu[s][:].rearrange("p a k j -> p (a k j)"), wgu_r[el, f], B_wgu[s], w=[B_wgu[s]])
            return s
        slots = {}
        slots[0] = load_gu(0)
        slots[1] = load_gu(1)
        for f in range(NF):
            if f + 2 < NF:
                slots[f + 2] = load_gu(f + 2)
            s = slots[f]
            for half in range(2):
                bg = (f * 2 + half) % 2 * 2
                for gu in range(2):
                    for k in range(16):
                        fw.op(pe, lambda gu=gu, k=k, s=s, half=half, bg=bg: T.matmul(
                            PS[bg + gu][:, :], wgu[s][:, gu, k, :], xinT[:, k, half * 512:(half + 1) * 512],
                            start=(k == 0), stop=(k == 15)),
                            r=[B_wgu[s], B_xin], w=[BPS[bg + gu]] if k == 0 else (),
                            pw=[BPS[bg + gu]] if k else (), inc=(k == 15))
                sgi = (f * 2 + half) % 2
                fw.op(act, lambda bg=bg, sgi=sgi: A.activation(out=sg[sgi][:], in_=PS[bg][:, :], func=AF.Silu),
                      r=[BPS[bg]], w=[B_sg[sgi]])
                fw.op(dve, lambda bg=bg, sgi=sgi, f=f, half=half: V.tensor_tensor(
                    out=hT[:, f, half * 512:(half + 1) * 512], in0=PS[bg + 1][:, :], in1=sg[sgi][:], op=ALU.mult),
                    r=[BPS[bg + 1], B_sg[sgi]], pw=[B_hT[f]])
        def load_wd(q, f):
            i = wd_i[0]; wd_i[0] += 1
            s = i % NWD
            fw.dma(pool, wdn[s][:], wd_r[el, q, f], B_wdn[s], w=[B_wdn[s]])
            return s
        seq = [(q, f) for q in range(4) for f in range(NF)]
        dsl = {}
        PRE = NWD - 1
        for i in range(PRE):
            dsl[seq[i]] = load_wd(*seq[i])
        for i, (q, f) in enumerate(seq):
            if i + PRE < len(seq):
                dsl[seq[i + PRE]] = load_wd(*seq[i + PRE])
            s = dsl[(q, f)]
            for st in range(8):
                fw.op(pe, lambda st=st, s=s, f=f: T.matmul(
                    PS[st][:, :], hT[:, f, st * 128:(st + 1) * 128], wdn[s][:], start=(f == 0), stop=(f == NF - 1)),
                    r=[B_hT[f], B_wdn[s]], w=[BPS[st]] if f == 0 else (), pw=[BPS[st]] if f else (),
                    inc=(st == 7 or f == NF - 1))
            if f == NF - 1:
                for st in range(8):
                    b, sc = st // 4, st % 4
                    col = (el * 2 + b) * 4 + sc
                    yi = yst_i[0] % 4; yst_i[0] += 1
                    eng, EE = (dve, V) if st % 2 == 0 else (act, A)
                    if st % 2 == 0:
                        fw.op(dve, lambda st=st, yi=yi, col=col: V.tensor_scalar(
                            out=yst[yi][:], in0=PS[st][:, :], scalar1=gate[:, col:col + 1], scalar2=None,
                            op0=ALU.mult), r=[BPS[st], B_gate], w=[B_yst[yi]])
                    else:
                        fw.op(act, lambda st=st, yi=yi, col=col: A.activation(
                            out=yst[yi][:], in_=PS[st][:, :], func=AF.Identity, scale=gate[:, col:col + 1]),
                            r=[BPS[st], B_gate], w=[B_yst[yi]])
                    row0 = el * 1024 + st * 128
                    fw.dma(sp, yloc[row0:row0 + 128, q * 512:(q + 1) * 512], yst[yi][:], B_yst[yi],
                           r=[B_yst[yi]], pw=[B_yloc])
    fw.coll(yloc[:, :], yall[:, :], r=[B_yloc], w=[B_yall])
    fw.barrier()
    sB.close()

    sC = contextlib.ExitStack()
    g2 = sbt(sC, "g2", [128, D], F32); b2 = sbt(sC, "b2", [128, D], F32); B_ln2 = Buf("ln2")
    ipm_t = [sbt(sC, f"ipm{c}", [128, 1], I32) for c in range(64)]; B_idxPM = Buf("idxPM")
    pm = [sbt(sC, f"pm{i}", [128, 8, 4], F32) for i in range(2)]; B_pm = [Buf("pm0"), Buf("pm1")]
    pmi = [sbt(sC, f"pmi{i}", [128, 8, 2], I32) for i in range(2)]; B_pmi = [Buf("pmi0"), Buf("pmi1")]
    acc_t = [sbt(sC, f"acc{i}", [128, D], F32) for i in range(2)]; B_acc = [Buf("acc0"), Buf("acc1")]
    xo = [sbt(sC, f"xo{i}", [128, D], F32) for i in range(2)]; B_xo = [Buf("xo0"), Buf("xo1")]
    NG = 4
    G = [sbt(sC, f"G{i}", [128, D], F32) for i in range(NG)]; B_G = [Buf(f"G{i}") for i in range(NG)]
    stats = sbt(sC, "stats2", [128, 4, 6], F32); B_stats = Buf("stats2")
    mv = sbt(sC, "mv2", [128, 8], F32); B_mv = Buf("mv2")
    fw.dma(sp, g2[:], ln2g[:, :], B_ln2, pw=[B_ln2])
    fw.dma(sp, b2[:], ln2b[:, :], B_ln2, pw=[B_ln2])
    for c in range(64):
        fw.dma(sp, ipm_t[c][:], idxPM_d[c], B_idxPM, pw=[B_idxPM])
    gi = [0]
    icolC = [sbt(sC, f"icolC{i}", [128, 1], I32) for i in range(6)]; B_icolC = [Buf(f"icolC{i}") for i in range(6)]
    icc_i = [0]
    for tt in range(8):
        p_, Bp_ = pm[tt % 2], B_pm[tt % 2]
        pi_, Bpi_ = pmi[tt % 2], B_pmi[tt % 2]
        ac, Bac = acc_t[tt % 2], B_acc[tt % 2]
        xo_, Bxo_ = xo[tt % 2], B_xo[tt % 2]
        for c8 in range(8):
            fw.dma(pool, p_[:, c8, :], pmall[:, :], Bp_, r=[B_pmall, B_idxPM], pw=[Bp_],
                   indirect=ipm_t[tt * 8 + c8][:, :])
        fw.op(dve, lambda p_=p_, pi_=pi_: V.tensor_copy(out=pi_[:], in_=p_[:, :, 0:2]), r=[Bp_], w=[Bpi_])
        fw.dma(sp, xo_[:], x1f[tt * 128:(tt + 1) * 128, :], Bxo_, r=[B_x1f], w=[Bxo_])
        fw.op(dve, lambda ac=ac, xo_=xo_: V.tensor_scalar(out=ac[:], in0=xo_[:], scalar1=ALPHA, scalar2=None,
                                                         op0=ALU.mult), r=[Bxo_], w=[Bac])
        for c8 in range(8):
            for el in range(2):
                i = gi[0] % NG; gi[0] += 1
                ii = icc_i[0] % 6; icc_i[0] += 1
                fw.op(dve, lambda ii=ii, pi_=pi_, c8=c8, el=el: V.tensor_copy(out=icolC[ii][:], in_=pi_[:, c8, el:el + 1]),
                      r=[Bpi_], w=[B_icolC[ii]])
                harden(icolC[ii], B_icolC[ii])
                fw.dma(pool, G[i][:], yall[:, :], B_G[i], r=[B_yall, B_icolC[ii]], w=[B_G[i]],
                       indirect=icolC[ii][:, :])
                fw.op(dve, lambda i=i, ac=ac, p_=p_, c8=c8, el=el: V.scalar_tensor_tensor(
                    out=ac[:], in0=G[i][:], scalar=p_[:, c8, 2 + el:3 + el], in1=ac[:], op0=ALU.mult, op1=ALU.add),
                    r=[B_G[i], Bp_], w=[Bac])

        def ln2(r_t, B_r, out_t, B_out):
            for c4 in range(4):
                fw.op(dve, lambda c4=c4: V.bn_stats(out=stats[:, c4, :], in_=r_t[:, c4 * 512:(c4 + 1) * 512]),
                      r=[B_r], pw=[B_stats])
            fw.op(dve, lambda: V.bn_aggr(out=mv[:, 0:2], in_=stats[:].rearrange("p a b -> p (a b)")),
                  r=[B_stats], pw=[B_mv])
            fw.op(dve, lambda: V.tensor_scalar(out=mv[:, 2:3], in0=mv[:, 1:2], scalar1=LN_EPS, scalar2=None,
                                               op0=ALU.add), r=[B_mv], pw=[B_mv])
            fw.op(act, lambda: A.sqrt(out=mv[:, 3:4], in_=mv[:, 2:3]), r=[B_mv], pw=[B_mv])
            fw.op(dve, lambda: V.reciprocal(out=mv[:, 4:5], in_=mv[:, 3:4]), r=[B_mv], pw=[B_mv])
            fw.op(dve, lambda: V.tensor_scalar(out=r_t[:], in0=r_t[:], scalar1=mv[:, 0:1], scalar2=mv[:, 4:5],
                                               op0=ALU.subtract, op1=ALU.mult), r=[B_mv], w=[B_r])
            fw.op(dve, lambda: V.tensor_tensor(out=r_t[:], in0=r_t[:], in1=g2[:], op=ALU.mult),
                  r=[B_ln2], w=[B_r])
            fw.op(dve, lambda: V.tensor_tensor(out=out_t[:], in0=r_t[:], in1=b2[:], op=ALU.add),
                  r=[B_ln2, B_r], w=[B_out])
        ln2(ac, Bac, xo_, Bxo_)
        fw.dma(sp, out_d[tt * 128:(tt + 1) * 128, :], xo_[:], Bxo_, r=[Bxo_])
    fw.barrier()
    sC.close()
    top.close()
    return nc


def _consts():
    c = {}
    Rm = np.zeros((128, 128), np.float32)
    for m in range(128):
        if (m % 64) < 32:
            Rm[m + 32, m] = -1.0
        else:
            Rm[m - 32, m] = 1.0
    c["Rm"] = Rm
    c["ident"] = np.eye(128, dtype=np.float32)
    c["ltri"] = np.triu(np.ones((128, 128), np.float32), 1)
    c["ones"] = np.ones((128, 128), np.float32)
    c["iota512"] = np.tile(np.arange(512, dtype=np.float32)[None, :], (128, 1))
    td = np.zeros((128, 2, 32, 2), np.float32)
    for b in range(2):
        t = b * S + np.arange(128)[:, None] * 32 + np.arange(32)[None, :]
        td[:, b, :, 0] = t // 64
        td[:, b, :, 1] = t % 64
    c["tdec"] = td.reshape(128, -1)
    return c


def _rope_tables(pos):
    inv = (10000.0 ** (-np.arange(0, 64, 2, dtype=np.float32) / np.float32(64))).astype(np.float32)
    ang = pos.astype(np.float32)[:, None] * inv[None, :]
    ang = np.concatenate([ang, ang], axis=-1)
    cos = np.cos(ang).astype(np.float32); sin = np.sin(ang).astype(np.float32)
    cosT = np.concatenate([cos.T, cos.T], axis=0)
    sinT = np.concatenate([sin.T, sin.T], axis=0)
    return np.ascontiguousarray(cosT), np.ascontiguousarray(sinT)


def _invcnt(t0):
    out = np.zeros((4, TOK), np.float32)
    t = t0 + np.arange(TOK)
    for gi, w in enumerate((2, 4, 8, 16)):
        lo = np.clip(t - w // 2, 0, S); hi = np.clip(t + w - w // 2, 0, S)
        out[gi] = 1.0 / (hi - lo).astype(np.float32)
    return np.tile(out.reshape(1, -1), (128, 1))


_PROG = {}


def make_inputs(x, w_in, pool_w, pool_scale, lambda_q1, lambda_k1, lambda_q2, lambda_k2, subln_g, w_out,
                ln1_g, ln1_b, w_router, w_gate, w_up, w_down, ln2_g, ln2_b):
    f = np.float32
    x = np.asarray(x, f); w_in = np.asarray(w_in, f)[0]
    cst = _consts()
    rep = lambda v: np.ascontiguousarray(np.tile(np.asarray(v, f).reshape(1, -1), (128, 1)))
    shared = dict(cst)
    shared["w_in_r"] = np.ascontiguousarray(
        w_in.reshape(16, 128, 32, 128).transpose(2, 1, 0, 3).reshape(32, 128, 2048))
    pw = np.asarray(pool_w, f)[0]
    shared["poolw_r"] = np.ascontiguousarray(pw.reshape(4, 2, 128, 256).transpose(2, 0, 1, 3).reshape(128, 2048))
    shared["pscale"] = np.ascontiguousarray(np.asarray(pool_scale, f)[0].reshape(8, 128).T)
    shared["lam_in"] = rep(np.concatenate([np.asarray(lambda_q1, f)[0], np.asarray(lambda_k1, f)[0],
                                           np.asarray(lambda_q2, f)[0], np.asarray(lambda_k2, f)[0]]))
    shared["subg"] = rep(np.asarray(subln_g, f)[0])
    shared["subgc"] = np.ascontiguousarray(np.asarray(subln_g, f)[0].reshape(128, 1))
    shared["w_out_r"] = np.ascontiguousarray(
        np.asarray(w_out, f)[0].reshape(16, 128, D).transpose(1, 0, 2).reshape(128, 16 * D))
    shared["ln1g"] = rep(np.asarray(ln1_g, f)[0]); shared["ln1b"] = rep(np.asarray(ln1_b, f)[0])
    shared["ln2g"] = rep(np.asarray(ln2_g, f)[0]); shared["ln2b"] = rep(np.asarray(ln2_b, f)[0])
    shared["wr_r"] = np.ascontiguousarray(
        np.asarray(w_router, f)[0].reshape(16, 128, 16).transpose(1, 0, 2).reshape(128, 256))
    wg = np.asarray(w_gate, f)[0]; wu = np.asarray(w_up, f)[0]; wd = np.asarray(w_down, f)[0]
    in_maps = []
    for c in range(NCORES):
        b, j = c // 4, c % 4
        t0 = j * TOK
        m = dict(shared)
        xp = np.zeros((TOK + 16, D), f)
        lo, hi = t0 - 8, t0 + TOK + 8
        slo, shi = max(lo, 0), min(hi, S)
        xp[slo - lo:shi - lo] = x[b, slo:shi]
        m["xT"] = np.ascontiguousarray(xp.T).reshape(16, 128, TOK + 16)
        m["xtok"] = np.ascontiguousarray(x[b, t0:t0 + TOK])
        m["cosT"], m["sinT"] = _rope_tables(np.arange(t0, t0 + TOK))
        m["invcnt"] = _invcnt(t0)
        idx = np.zeros((128, 64), np.int32)
        p = np.arange(128)
        for h in range(8):
            for r in range(4):
                idx[:, h * 4 + r] = (4 * b + r) * 2048 + h * 128 + p
                idx[:, 32 + h * 4 + r] = (4 * b + r) * 2048 + 1024 + h * 128 + p
        m["idxKV"] = np.ascontiguousarray(idx.T).reshape(64, 128, 1)
        sel = np.zeros((128, 2, 16), f)
        sel[:, 0, 2 * c] = 1.0; sel[:, 1, 2 * c + 1] = 1.0
        m["sel"] = sel.reshape(128, 32)
        m["coreoff"] = np.full((128, 1), c * 2048, f)
        ipm = np.zeros((128, 64), np.int32)
        for tt in range(8):
            for c8 in range(8):
                ipm[:, tt * 8 + c8] = c8 * 2 * S + c * TOK + tt * 128 + p
        m["idxPM"] = np.ascontiguousarray(ipm.T).reshape(64, 128, 1)
        wgu = np.empty((2, NF, 128, 2, 16, 128), f)
        wdr = np.empty((2, 4, NF, 128, 512), f)
        for el in range(2):
            e = 2 * c + el
            wgu[el, :, :, 0] = wg[e].reshape(16, 128, NF, 128).transpose(2, 1, 0, 3)
            wgu[el, :, :, 1] = wu[e].reshape(16, 128, NF, 128).transpose(2, 1, 0, 3)
            wdr[el] = wd[e].reshape(NF, 128, 4, 512).transpose(2, 0, 1, 3)
        m["wgu_r"] = wgu.reshape(2, NF, 128, 4096)
        m["wd_r"] = wdr
        in_maps.append(m)
    return in_maps


def kernel(**inputs):
    in_maps = make_inputs(**inputs)
    if "nc" not in _PROG:
        _PROG["nc"] = build_program()
    res = run_bass_kernel_spmd(_PROG["nc"], in_maps, core_ids=list(range(NCORES)))
    out = np.concatenate([np.asarray(res.results[c]["out"], np.float32) for c in range(NCORES)], axis=0)
    return out.reshape(2, S, D)
```
